# Optimizing a Trainium2 kernel written in Bass

```python
import math
import jax, jax.numpy as jnp
from jax import lax
import numpy as np

D_MODEL = 1024
BATCH = 8
SEQ = 2048
DEPTH = 2

HEAD_DIM = 64
POOL_DIM = D_MODEL // 4
POOL_WINDOWS = (2, 4, 8, 16)
POOL_GROUP = POOL_DIM // len(POOL_WINDOWS)
N_ATT_HEADS = (3 * D_MODEL // 8) // HEAD_DIM
N_KV_HEADS = 2
GQA_GROUP = N_ATT_HEADS // N_KV_HEADS
ATT_DIM = N_ATT_HEADS * HEAD_DIM
KV_DIM = N_KV_HEADS * HEAD_DIM
WINDOW = 128
N_BUCKETS = 32
MAX_DISTANCE = 128
N_GDN_HEADS = (3 * D_MODEL // 8) // HEAD_DIM
GDN_DIM = N_GDN_HEADS * HEAD_DIM
CONV_WIDTH = 4
GDN_CHUNK = 64
MIX_DIM = POOL_DIM + ATT_DIM + GDN_DIM
IN_DIM = POOL_DIM + ATT_DIM + 2 * KV_DIM + 4 * GDN_DIM + 2 * N_GDN_HEADS
N_EXPERTS = 32
TOP_K = 4
EXPERT_DIM = D_MODEL
SWIGLU_ALPHA = 1.702
SWIGLU_LIMIT = 7.0
MOE_BLOCK = 256
DEEPNORM_ALPHA = (2 * DEPTH) ** 0.25
DEEPNORM_BETA = (8 * DEPTH) ** -0.25
LN_EPS = 1e-5
NORM_EPS = 1e-6
NEG_INF = -1e30

kernel_name = "hybrid_pool_swa_gdn_moe_deepnorm"


def layer_norm(x, g, b):
    xf = x.astype(jnp.float32)
    mu = xf.mean(-1, keepdims=True)
    var = jnp.square(xf - mu).mean(-1, keepdims=True)
    y = (xf - mu) * lax.rsqrt(var + LN_EPS) * g.astype(jnp.float32) + b.astype(jnp.float32)
    return y.astype(x.dtype)


def t5_bucket(n):
    max_exact = N_BUCKETS // 2
    nf = jnp.maximum(n, 1).astype(jnp.float32)
    large = max_exact + (jnp.log(nf / max_exact) / math.log(MAX_DISTANCE / max_exact)
                         * (N_BUCKETS - max_exact)).astype(jnp.int32)
    large = jnp.minimum(large, N_BUCKETS - 1)
    return jnp.where(n < max_exact, n, large)


def pool_mixer(u, pool_w, pool_scale):
    Bb, S_, _ = u.shape
    uf = u.astype(jnp.float32)
    cnt_base = jnp.arange(1, S_ + 1, dtype=jnp.float32)
    outs = []
    for gi, w in enumerate(POOL_WINDOWS):
        ug = uf[..., gi * POOL_GROUP:(gi + 1) * POOL_GROUP]
        cs = jnp.cumsum(ug, axis=1)
        shifted = jnp.pad(cs, ((0, 0), (w, 0), (0, 0)))[:, :S_]
        cnt = jnp.minimum(cnt_base, float(w))
        outs.append((cs - shifted) / cnt[None, :, None] - ug)
    p = jnp.stack(outs, axis=2)
    y = jnp.einsum('bsgc,gcd->bsgd', p, pool_w.astype(jnp.float32)).reshape(Bb, S_, POOL_DIM)
    return (y * pool_scale.astype(jnp.float32)).astype(u.dtype)


def swa_attention(q, k, v, sinks, rel_bias):
    Bb, S_ = q.shape[:2]
    NB = S_ // WINDOW
    qf, kf, vf = (t.astype(jnp.float32) for t in (q, k, v))
    qb = qf.reshape(Bb, NB, WINDOW, N_KV_HEADS, GQA_GROUP, HEAD_DIM)

    def band(t):
        tp = jnp.pad(t, ((0, 0), (WINDOW, 0), (0, 0), (0, 0)))
        prev = tp[:, :S_].reshape(Bb, NB, WINDOW, N_KV_HEADS, HEAD_DIM)
        cur = t.reshape(Bb, NB, WINDOW, N_KV_HEADS, HEAD_DIM)
        return jnp.concatenate([prev, cur], axis=2)

    kb, vb = band(kf), band(vf)
    s = jnp.einsum('bnqhgd,bnkhd->bnhgqk', qb, kb) * (HEAD_DIM ** -0.5)
    qi = jnp.arange(WINDOW)[:, None]
    kj = jnp.arange(2 * WINDOW)[None, :]
    dist = qi + WINDOW - kj
    in_band = (dist >= 0) & (dist < WINDOW)
    key_abs = jnp.arange(NB)[:, None] * WINDOW - WINDOW + kj
    valid = in_band[None] & (key_abs >= 0)[:, None, :]
    bias = rel_bias.astype(jnp.float32)[t5_bucket(jnp.maximum(dist, 0))]
    bias = bias.transpose(2, 0, 1).reshape(N_KV_HEADS, GQA_GROUP, WINDOW, 2 * WINDOW)
    s = jnp.where(valid[None, :, None, None], s + bias[None, None], NEG_INF)
    sink = sinks.astype(jnp.float32).reshape(N_KV_HEADS, GQA_GROUP)[None, None, :, :, None, None]
    m = jnp.maximum(s.max(-1, keepdims=True), sink)
    p = jnp.exp(s - m)
    den = p.sum(-1, keepdims=True) + jnp.exp(sink - m)
    o = jnp.einsum('bnhgqk,bnkhd->bnqhgd', p / den, vb)
    return o.reshape(Bb, S_, ATT_DIM).astype(q.dtype)


def gated_delta_chunked(q, k, v, g, beta):
    Bb, S_, H, Dk = q.shape
    Dv = v.shape[-1]
    N = S_ // GDN_CHUNK
    C = GDN_CHUNK
    q = q * (Dk ** -0.5)

    def chunks(t):
        return t.reshape(Bb, N, C, H, -1).transpose(0, 3, 1, 2, 4)

    qc, kc, vc = chunks(q), chunks(k), chunks(v)
    gc = g.reshape(Bb, N, C, H).transpose(0, 3, 1, 2)
    bc = beta.reshape(Bb, N, C, H).transpose(0, 3, 1, 2)
    gcum = jnp.cumsum(gc, axis=-1)
    causal = jnp.tril(jnp.ones((C, C), dtype=bool))
    strict = jnp.tril(jnp.ones((C, C), dtype=jnp.float32), -1)
    decay = jnp.exp(jnp.where(causal, gcum[..., :, None] - gcum[..., None, :], -jnp.inf))
    kbeta = kc * bc[..., None]
    vbeta = vc * bc[..., None]
    L = jnp.einsum('bhnid,bhnjd->bhnij', kbeta, kc) * decay * strict
    a_mat = jnp.eye(C, dtype=jnp.float32) + L
    rhs = jnp.concatenate([vbeta, kbeta * jnp.exp(gcum)[..., None]], axis=-1)
    sol = lax.linalg.triangular_solve(a_mat, rhs, left_side=True, lower=True, unit_diagonal=True)
    u, w = sol[..., :Dv], sol[..., Dv:]
    attn = jnp.einsum('bhnid,bhnjd->bhnij', qc, kc) * decay
    q_dec = qc * jnp.exp(gcum)[..., None]
    k_dec = kc * jnp.exp(gcum[..., -1:] - gcum)[..., None]
    chunk_decay = jnp.exp(gcum[..., -1])

    def step(state, inp):
        u_n, w_n, qd_n, kd_n, a_n, cd_n = inp
        v_new = u_n - jnp.einsum('bhck,bhkv->bhcv', w_n, state)
        o_n = jnp.einsum('bhck,bhkv->bhcv', qd_n, state) + jnp.einsum('bhij,bhjv->bhiv', a_n, v_new)
        state = state * cd_n[..., None, None] + jnp.einsum('bhck,bhcv->bhkv', kd_n, v_new)
        return state, o_n

    xs = tuple(jnp.moveaxis(t, 2, 0) for t in (u, w, q_dec, k_dec, attn, chunk_decay))
    state0 = jnp.zeros((Bb, H, Dk, Dv), jnp.float32)
    _, o = lax.scan(step, state0, xs)
    return o.transpose(1, 0, 3, 2, 4).reshape(Bb, S_, H, Dv)


def gdn_mixer(qkv, z, b_raw, a_raw, conv_w, a_log, dt_bias, norm_w):
    Bb, S_, Cc = qkv.shape
    conv = lax.conv_general_dilated(
        qkv, conv_w.astype(qkv.dtype)[:, None, :], window_strides=(1,),
        padding=[(CONV_WIDTH - 1, 0)], dimension_numbers=('NWC', 'WIO', 'NWC'),
        feature_group_count=Cc)
    conv = jax.nn.silu(conv.astype(jnp.float32))
    q, k, v = (t.reshape(Bb, S_, N_GDN_HEADS, HEAD_DIM) for t in jnp.split(conv, 3, axis=-1))
    q = q * lax.rsqrt(jnp.sum(q * q, -1, keepdims=True) + NORM_EPS)
    k = k * lax.rsqrt(jnp.sum(k * k, -1, keepdims=True) + NORM_EPS)
    beta = jax.nn.sigmoid(b_raw.astype(jnp.float32))
    g = -jnp.exp(a_log.astype(jnp.float32)) * jax.nn.softplus(a_raw.astype(jnp.float32) + dt_bias.astype(jnp.float32))
    o = gated_delta_chunked(q, k, v, g, beta)
    o = o * lax.rsqrt(jnp.mean(o * o, -1, keepdims=True) + NORM_EPS) * norm_w.astype(jnp.float32)
    o = o * jax.nn.silu(z.astype(jnp.float32).reshape(Bb, S_, N_GDN_HEADS, HEAD_DIM))
    return o.reshape(Bb, S_, GDN_DIM).astype(qkv.dtype)


def hybrid_mixer(h, w_in, w_out, pool_w, pool_scale, sinks, rel_bias, conv_w, a_log, dt_bias, norm_w):
    Bb, S_, _ = h.shape
    proj = h @ w_in.astype(h.dtype)
    sizes = (POOL_DIM, ATT_DIM, KV_DIM, KV_DIM, 3 * GDN_DIM, GDN_DIM, N_GDN_HEADS, N_GDN_HEADS)
    cuts = tuple(int(i) for i in np.cumsum(sizes)[:-1])
    u_pool, aq, ak, av, gqkv, gz, gb, ga = jnp.split(proj, cuts, axis=-1)
    y_pool = pool_mixer(u_pool, pool_w, pool_scale)
    y_att = swa_attention(aq.reshape(Bb, S_, N_ATT_HEADS, HEAD_DIM),
                          ak.reshape(Bb, S_, N_KV_HEADS, HEAD_DIM),
                          av.reshape(Bb, S_, N_KV_HEADS, HEAD_DIM), sinks, rel_bias)
    y_gdn = gdn_mixer(gqkv, gz, gb, ga, conv_w, a_log, dt_bias, norm_w)
    y = jnp.concatenate([y_pool, y_att, y_gdn], axis=-1)
    return y @ w_out.astype(h.dtype)


def moe_ffn(h, router_w, router_b, w_up, b_up, w_down, b_down):
    Bb, S_, D = h.shape
    T = Bb * S_
    A = T * TOP_K
    xt = h.reshape(T, D)
    logits = xt.astype(jnp.float32) @ router_w.astype(jnp.float32) + router_b.astype(jnp.float32)
    top_vals, top_idx = lax.top_k(logits, TOP_K)
    gates = jax.nn.softmax(top_vals, axis=-1)
    flat_e = top_idx.reshape(A)
    order = jnp.argsort(flat_e, stable=True)
    sorted_e = flat_e[order]
    counts = jnp.bincount(flat_e, length=N_EXPERTS)
    padded = ((counts + MOE_BLOCK - 1) // MOE_BLOCK) * MOE_BLOCK
    start = jnp.cumsum(counts) - counts
    pcum = jnp.cumsum(padded)
    pstart = pcum - padded
    dest_sorted = pstart[sorted_e] + (jnp.arange(A) - start[sorted_e])
    dest = jnp.zeros((A,), jnp.int32).at[order].set(dest_sorted.astype(jnp.int32))
    n_blocks = -(-A // MOE_BLOCK) + N_EXPERTS
    P = n_blocks * MOE_BLOCK
    tok = jnp.arange(A) // TOP_K
    xbuf = jnp.zeros((P, D), h.dtype).at[dest].set(xt[tok])
    block_e = jnp.minimum(jnp.searchsorted(pcum, jnp.arange(n_blocks) * MOE_BLOCK, side='right'),
                          N_EXPERTS - 1)

    def expert_block(args):
        xb, e = args
        hb = xb @ w_up[e].astype(xb.dtype) + b_up[e].astype(xb.dtype)
        x_glu = jnp.minimum(hb[:, :EXPERT_DIM], SWIGLU_LIMIT)
        x_lin = jnp.clip(hb[:, EXPERT_DIM:], -SWIGLU_LIMIT, SWIGLU_LIMIT)
        act = x_glu * jax.nn.sigmoid(SWIGLU_ALPHA * x_glu) * (x_lin + 1)
        return act @ w_down[e].astype(xb.dtype) + b_down[e].astype(xb.dtype)

    ybuf = lax.map(expert_block, (xbuf.reshape(n_blocks, MOE_BLOCK, D), block_e)).reshape(P, D)
    y = ybuf[dest].reshape(T, TOP_K, D)
    out = jnp.einsum('tk,tkd->td', gates.astype(y.dtype), y)
    return out.reshape(Bb, S_, D)


def setup_inputs(seed: int = 0) -> dict:
    key = jax.random.key(seed)
    ks = jax.random.split(key, 26)
    f32 = jnp.float32

    def nrm(k, shape, s):
        return jax.random.normal(k, shape, f32) * s

    dt = jnp.exp(jax.random.uniform(ks[15], (DEPTH, N_GDN_HEADS), f32, math.log(1e-3), math.log(1e-1)))
    return {
        "x": nrm(ks[0], (BATCH, SEQ, D_MODEL), 1.0),
        "c": nrm(ks[1], (BATCH, D_MODEL), 1.0),
        "rel_bias": nrm(ks[2], (N_BUCKETS, N_ATT_HEADS), 0.3),
        "w_in": nrm(ks[3], (DEPTH, D_MODEL, IN_DIM), D_MODEL ** -0.5),
        "w_out": nrm(ks[4], (DEPTH, MIX_DIM, D_MODEL), MIX_DIM ** -0.5 * DEEPNORM_BETA),
        "w_ada": nrm(ks[5], (DEPTH, D_MODEL, 6 * D_MODEL), D_MODEL ** -0.5),
        "b_ada": nrm(ks[6], (DEPTH, 6 * D_MODEL), 0.02),
        "ln1_g": 1.0 + nrm(ks[7], (DEPTH, D_MODEL), 0.02),
        "ln1_b": nrm(ks[8], (DEPTH, D_MODEL), 0.02),
        "ln2_g": 1.0 + nrm(ks[9], (DEPTH, D_MODEL), 0.02),
        "ln2_b": nrm(ks[10], (DEPTH, D_MODEL), 0.02),
        "pool_w": nrm(ks[11], (DEPTH, len(POOL_WINDOWS), POOL_GROUP, POOL_GROUP), POOL_GROUP ** -0.5),
        "pool_scale": 1.0 + nrm(ks[12], (DEPTH, POOL_DIM), 0.02),
        "attn_sinks": nrm(ks[13], (DEPTH, N_ATT_HEADS), 1.0),
        "conv_w": nrm(ks[14], (DEPTH, CONV_WIDTH, 3 * GDN_DIM), CONV_WIDTH ** -0.5),
        "gdn_a_log": jnp.log(jax.random.uniform(ks[16], (DEPTH, N_GDN_HEADS), f32, 1.0, 16.0)),
        "gdn_dt_bias": dt + jnp.log(-jnp.expm1(-dt)),
        "gdn_norm_w": 1.0 + nrm(ks[17], (DEPTH, HEAD_DIM), 0.02),
        "router_w": nrm(ks[18], (DEPTH, D_MODEL, N_EXPERTS), D_MODEL ** -0.5),
        "router_b": nrm(ks[19], (DEPTH, N_EXPERTS), 0.01),
        "exp_w_up": nrm(ks[20], (DEPTH, N_EXPERTS, D_MODEL, 2 * EXPERT_DIM), D_MODEL ** -0.5),
        "exp_b_up": nrm(ks[21], (DEPTH, N_EXPERTS, 2 * EXPERT_DIM), 0.01),
        "exp_w_down": nrm(ks[22], (DEPTH, N_EXPERTS, EXPERT_DIM, D_MODEL), EXPERT_DIM ** -0.5 * DEEPNORM_BETA),
        "exp_b_down": nrm(ks[23], (DEPTH, N_EXPERTS, D_MODEL), 0.01),
    }


def reference(x, c, rel_bias, w_in, w_out, w_ada, b_ada, ln1_g, ln1_b, ln2_g, ln2_b,
              pool_w, pool_scale, attn_sinks, conv_w, gdn_a_log, gdn_dt_bias, gdn_norm_w,
              router_w, router_b, exp_w_up, exp_b_up, exp_w_down, exp_b_down):
    c_act = jax.nn.silu(c)
    for l in range(DEPTH):
        mod = (c_act @ w_ada[l] + b_ada[l]).astype(x.dtype)
        sh1, sc1, g1, sh2, sc2, g2 = (m[:, None, :] for m in jnp.split(mod, 6, axis=-1))
        h = x * (1 + sc1) + sh1
        y = hybrid_mixer(h, w_in[l], w_out[l], pool_w[l], pool_scale[l], attn_sinks[l], rel_bias,
                         conv_w[l], gdn_a_log[l], gdn_dt_bias[l], gdn_norm_w[l])
        x = layer_norm(DEEPNORM_ALPHA * x + g1 * y, ln1_g[l], ln1_b[l])
        h = x * (1 + sc2) + sh2
        y = moe_ffn(h, router_w[l], router_b[l], exp_w_up[l], exp_b_up[l], exp_w_down[l], exp_b_down[l])
        x = layer_norm(DEEPNORM_ALPHA * x + g2 * y, ln2_g[l], ln2_b[l])
    return x
```

```python
import math
import os
import numpy as np
KSKIP = os.environ.get('KSKIP', '')
from contextlib import ExitStack
import concourse.bass as bass
import concourse.mybir as mybir
from concourse.bass_utils import run_bass_kernel_spmd

F32 = mybir.dt.float32
BF16 = mybir.dt.bfloat16
AF = mybir.ActivationFunctionType
ALU = mybir.AluOpType
AX = mybir.AxisListType

COMPUTE = ("pe", "act", "dve", "pool")
ALLQ = ("pe", "act", "dve", "pool", "sp")

D = 1024
IN_DIM = 2444
NE = 32
ALPHA = 4 ** 0.25
BIG = 30000.0


class Sched:
    def __init__(self, nc, stack):
        self.nc = nc
        self.stack = stack
        self.ops = []
        self.res = {}
        self.bar = set()
        self.lastq = {}
        self.dmas_since_bar = []
        self.alias = {}

    def _add(self, q, fn, reads, writes, dma=False):
        reads = tuple(self.alias.get(r, r) for r in reads)
        writes = tuple(self.alias.get(w, w) for w in writes)
        deps = set(self.bar)
        for r in reads:
            st = self.res.get(r)
            if st is not None and st[0] is not None:
                deps.add(st[0])
            if st is not None and isinstance(r, tuple) and r[0] in ("pf", "ph"):
                for rq, ro in st[1].items():
                    if rq != q:
                        deps.add(ro)
        for w in writes:
            st = self.res.get(w)
            if st is not None:
                if st[0] is not None:
                    deps.add(st[0])
                deps.update(st[1].values())
                deps.update(st[2])
        oid = len(self.ops)
        self.ops.append(dict(q=q, fn=fn, deps=deps, dma=dma))
        for r in reads:
            st = self.res.setdefault(r, [None, {}, []])
            if dma:
                st[2].append(oid)
            else:
                st[1][q] = oid
        for w in writes:
            self.res[w] = [oid, {}, []]
        if dma:
            self.dmas_since_bar.append(oid)
        else:
            self.lastq[q] = oid
        return oid

    def op(self, q, fn, reads=(), writes=()):
        return self._add(q, fn, tuple(reads), tuple(writes), dma=False)

    def dma(self, q, out, in_, reads=(), writes=(), slow=False):
        def fn(eng):
            if slow:
                return eng.dma_start(out=out, in_=in_, allow_slow_non_contiguous=True)
            return eng.dma_start(out=out, in_=in_)
        return self._add(q, fn, tuple(reads), tuple(writes), dma=True)

    def barrier(self):
        self.bar = set(self.lastq.values()) | set(self.dmas_since_bar) | set(self.bar)
        self.dmas_since_bar = []

    def emit(self):
        nc = self.nc
        ops = self.ops
        needed = set()
        for o in ops:
            needed.update(o["deps"])
        NLANE = 24
        sems = {q: self.stack.enter_context(nc.semaphore("s_" + q)) for q in COMPUTE}
        lanes = [self.stack.enter_context(nc.semaphore("l_%d" % i)) for i in range(NLANE)]
        lane_val = [0] * NLANE
        cnt = {q: 0 for q in COMPUTE}
        tok = {}
        li = 0
        lane_prev = {}
        for i, o in enumerate(ops):
            if o["dma"]:
                lane = li % NLANE
                li += 1
                lane_val[lane] += 16
                tok[i] = (("L", lane), lane_val[lane])
                if lane in lane_prev:
                    o["deps"] = set(o["deps"]) | {lane_prev[lane]}
                lane_prev[lane] = i
            elif i in needed:
                cnt[o["q"]] += 1
                tok[i] = (("E", o["q"]), cnt[o["q"]])

        def semof(k):
            return lanes[k[1]] if k[0] == "L" else sems[k[1]]

        per_q = {q: [] for q in ALLQ}
        for i, o in enumerate(ops):
            per_q[o["q"]].append(i)
        block = self.stack.enter_context(nc.Block())
        engmap = {}

        def run_q(q, eng):
            seen = {}
            for i in per_q[q]:
                o = ops[i]
                want = {}
                for d in o["deps"]:
                    if d not in tok:
                        continue
                    k, v = tok[d]
                    if (not ops[d]["dma"]) and ops[d]["q"] == q and q == "pe":
                        continue
                    if want.get(k, 0) < v:
                        want[k] = v
                for k, v in want.items():
                    if seen.get(k, 0) < v:
                        eng.wait_ge(semof(k), v)
                        seen[k] = v
                inst = o["fn"](eng)
                if i in tok:
                    k, v = tok[i]
                    inst.then_inc(semof(k), 16 if o["dma"] else 1)
            last = {}
            for i in per_q[q]:
                if ops[i]["dma"]:
                    k, v = tok[i]
                    last[k] = max(last.get(k, 0), v)
            for k, v in last.items():
                if seen.get(k, 0) < v:
                    eng.wait_ge(semof(k), v)

        @block.tensor
        def _(e):
            run_q("pe", e)

        @block.scalar
        def _(e):
            run_q("act", e)

        @block.vector
        def _(e):
            run_q("dve", e)

        @block.gpsimd
        def _(e):
            run_q("pool", e)

        @block.sync
        def _(e):
            run_q("sp", e)


def t5_bucket_np(n):
    max_exact = 16
    nf = np.maximum(n, 1).astype(np.float32)
    large = max_exact + (np.log(nf / max_exact) / math.log(128 / max_exact) * (32 - max_exact)).astype(np.int32)
    large = np.minimum(large, 31)
    return np.where(n < max_exact, n, large)


CW_IDENT = 0
CW_ONES = 128
CW_U = 256
CW_NS = 320
CW_MB = 832
CW_I8 = 1344
CW_OH = 1856
CW_PF = 2240
CW_TOT = 2272


def make_consts():
    c = np.zeros((128, CW_TOT), np.float32)
    c[:, CW_IDENT:CW_IDENT + 128] = np.eye(128, dtype=np.float32)
    c[:, CW_ONES:CW_ONES + 128] = 1.0
    k = np.arange(64)[:, None]
    i = np.arange(64)[None, :]
    c[:64, CW_U:CW_U + 64] = (k <= i)
    ns = np.where(i > k, -1.0, 0.0)
    mb = np.where(i < k, -BIG, 0.0)
    c[:64, CW_NS:CW_NS + 512] = np.tile(ns, (1, 8))
    c[:64, CW_MB:CW_MB + 512] = np.tile(mb, (1, 8))
    c[:64, CW_I8:CW_I8 + 512] = np.tile(np.eye(64), (1, 8))
    dist = np.arange(384) - 127
    valid = (dist >= 0) & (dist < 128)
    bk = t5_bucket_np(np.maximum(dist, 0))
    oh = np.zeros((33, 384), np.float32)
    for ii in range(384):
        if valid[ii]:
            oh[bk[ii], ii] = 1.0
        else:
            oh[32, ii] = -BIG
    c[:33, CW_OH:CW_OH + 384] = oh
    t = np.arange(16)
    for ci, (wa, wb) in enumerate(((2, 4), (8, 16))):
        c[:64, CW_PF + ci * 16:CW_PF + ci * 16 + 16] = wa / np.minimum(t + 1, wa)
        c[64:, CW_PF + ci * 16:CW_PF + ci * 16 + 16] = wb / np.minimum(t + 1, wb)
    return c


WNAMES = [("rel_bias", [32, 6]), ("w_in", [2, D, IN_DIM]), ("w_out", [2, D, D]), ("w_ada", [2, D, 6 * D]),
          ("b_ada", [2, 6 * D]), ("ln1_g", [2, D]), ("ln1_b", [2, D]), ("ln2_g", [2, D]), ("ln2_b", [2, D]),
          ("pool_w", [2, 4, 64, 64]), ("pool_scale", [2, 256]), ("attn_sinks", [2, 6]), ("conv_w", [2, 4, 1152]),
          ("gdn_a_log", [2, 6]), ("gdn_dt_bias", [2, 6]), ("gdn_norm_w", [2, 64]), ("router_w", [2, D, NE]),
          ("router_b", [2, NE]), ("exp_w_up", [2, NE, D, 2 * D]), ("exp_b_up", [2, NE, 2 * D]),
          ("exp_w_down", [2, NE, D, D]), ("exp_b_down", [2, NE, D])]


def build(NG=8, NL=2, do_moe=True, n_exp=NE, ymask=(1, 1, 1)):
    T = NG * 256
    NT = T // 128
    nc = bass.Bass("TRN2", target_bir_lowering=False)
    dr = {}
    dr["x"] = nc.dram_tensor("x", [T, D], F32, kind="ExternalInput").ap()
    dr["c"] = nc.dram_tensor("c", [1, D], F32, kind="ExternalInput").ap()
    dr["consts"] = nc.dram_tensor("consts", [128, CW_TOT], F32, kind="ExternalInput").ap()
    for nm, shp in WNAMES:
        if nm.startswith("exp_w") and not do_moe:
            continue
        dr[nm] = nc.dram_tensor(nm, shp, F32, kind="ExternalInput").ap()
    out = nc.dram_tensor("out", [T, D], F32, kind="ExternalOutput").ap()
    zscr = nc.dram_tensor("zscr", [6, 128, 384], F32, kind="ExternalOutput").ap()
    xs = nc.dram_tensor("xs", [T, D], F32, kind="ExternalOutput").ap()
    dbg = nc.dram_tensor("dbg", [128, 1536], F32, kind="ExternalOutput").ap() if not do_moe else None

    with ExitStack() as st:
        S = Sched(nc, st)
        base = [((nc.sbuf_base + 63) // 64) * 64]
        top = nc.sbuf_top

        acache = {}

        def alloc(name, shape, dt):
            nbytes = int(np.prod(shape[1:])) * (2 if dt == BF16 else 4)
            nbytes = (nbytes + 63) // 64 * 64
            off = base[0]
            base[0] += nbytes
            assert base[0] <= top, ("SBUF overflow", name, base[0], top)
            ck = (name, tuple(shape), str(dt), off)
            if ck not in acache:
                acache[ck] = nc.alloc_sbuf_tensor_at(name, list(shape), dt, offset=off)
            return acache[ck]

        pfall = st.enter_context(nc.psum_tensor("pfall", [128, 4096], F32))
        pf = [pfall[:, i * 512:(i + 1) * 512] for i in range(8)]
        rr = [0, 0]

        def pbank():
            i = rr[0] % 7
            rr[0] += 1
            return pf[i], ("pf", i)

        def pbank2():
            i = (rr[1] % 3) * 2
            rr[1] += 1
            return pfall[:, i * 512:(i + 2) * 512], [("pf", i), ("pf", i + 1)]

        def mm(out_, lhsT, rhs, start, stop, reads, writes):
            S.op("pe", lambda e: e.matmul(out_, lhsT=lhsT, rhs=rhs, start=start, stop=stop), reads, writes)

        def tr(out_, in_, ident_, reads, writes):
            S.op("pe", lambda e: e.transpose(out=out_, in_=in_, identity=ident_), reads, writes)

        def act(out_, in_, func, reads, writes, scale=None, bias=None):
            kw = {}
            if scale is not None:
                kw["scale"] = scale
            if bias is not None:
                kw["bias"] = bias
            S.op("act", lambda e: e.activation(out=out_, in_=in_, func=func, **kw), reads, writes)

        def tt(q, out_, a, b, op, reads, writes):
            S.op(q, lambda e: e.tensor_tensor(out=out_, in0=a, in1=b, op=op), reads, writes)

        def ts(q, out_, a, s1, s2, op0, op1, reads, writes):
            if op1 is None:
                S.op(q, lambda e: e.tensor_scalar(out=out_, in0=a, scalar1=s1, scalar2=None, op0=op0), reads, writes)
            else:
                S.op(q, lambda e: e.tensor_scalar(out=out_, in0=a, scalar1=s1, scalar2=s2, op0=op0, op1=op1), reads, writes)

        def stt(q, out_, a, sc, b, op0, op1, reads, writes):
            S.op(q, lambda e: e.scalar_tensor_tensor(out=out_, in0=a, scalar=sc, in1=b, op0=op0, op1=op1), reads, writes)

        def cp(q, out_, in_, reads, writes):
            S.op(q, lambda e: e.tensor_copy(out=out_, in_=in_), reads, writes)

        def memset(q, ap, val, writes):
            S.op(q, lambda e: e.memset(ap, val), (), writes)

        stg = {}
        stg_i = [0]

        def load_cast(dst, src, w, key, mul=None, srcs=None):
            i = stg_i[0] % len(stg["bufs"])
            stg_i[0] += 1
            sbuf = stg["bufs"][i]
            sk = "stg%d" % i
            if srcs is None:
                S.dma("sp", sbuf[:, :, 0:w], src.rearrange("(k p) n -> p k n", p=128), writes=[(sk, 0), (sk, 128)])
                sks = [(sk, 0), (sk, 128)]
            else:
                sks = []
                for (c0, ncol, ap) in srcs:
                    S.dma("sp", sbuf[:, :, c0:c0 + ncol], ap.rearrange("(k p) n -> p k n", p=128), writes=[(sk, c0)])
                    sks.append((sk, c0))
            if mul is None:
                if stg_i[0] % 2 == 0:
                    act(dst, sbuf[:, :, 0:w], AF.Copy, sks, [key])
                else:
                    cp("pool", dst, sbuf[:, :, 0:w], sks, [key])
                return [key]
            mb = mul.unsqueeze(1).to_broadcast([128, 4, w])
            tt("pool", dst[:, 0:4, :], sbuf[:, 0:4, 0:w], mb, ALU.mult, sks + ["gbc"], [(key, 0)])
            tt("dve", dst[:, 4:8, :], sbuf[:, 4:8, 0:w], mb, ALU.mult, sks + ["gbc"], [(key, 1)])
            return [(key, 0), (key, 1)]

        cst = alloc("cst", [128, CW_TOT], F32)
        ident = cst[:, CW_IDENT:CW_IDENT + 128]
        ones = cst[:, CW_ONES:CW_ONES + 128]
        U64 = cst[0:64, CW_U:CW_U + 64]
        NS8 = cst[0:64, CW_NS:CW_NS + 512]
        MB8 = cst[0:64, CW_MB:CW_MB + 512]
        I8 = cst[0:64, CW_I8:CW_I8 + 512]
        OH = cst[0:33, CW_OH:CW_OH + 384]
        identb = alloc("identb", [128, 128], BF16)
        ca = alloc("ca", [128, 8], F32)
        modcol = alloc("modcol", [128, 4, 8], F32)
        gbc = alloc("gbc", [128, 2, D], F32)
        rowt = alloc("rowt", [1, 1024], F32)
        rowm = alloc("rowm", [1, 512], F32)
        small = alloc("small", [128, 128], F32)
        phase_base = base[0]

        S.dma("sp", cst[:], dr["consts"][:, :], writes=["cst"])
        cp("dve", identb[:], ident, ["cst"], ["identb"])
        S.dma("sp", ca[:], dr["c"][0, :].rearrange("(k p) -> p k", p=128), writes=["ca"], slow=True)
        act(ca[:], ca[:], AF.Silu, ["ca"], ["ca"])

        def bcast_row(dst, src_row, n, key):
            S.dma("sp", rowt[0:1, 0:n], src_row, writes=["rowt"])
            for h0 in range(0, n, 512):
                w = min(512, n - h0)
                pb, pk = pbank()
                mm(pb[:, 0:w], ones[0:1, 0:128], rowt[0:1, h0:h0 + w], True, True, ["cst", "rowt"], [pk])
                act(dst[:, h0:h0 + w], pb[:, 0:w], AF.Copy, [pk], [key])

        rbT = alloc("rbT", [33, 6, 128], F32)
        rb = alloc("rb", [32, 6], F32)
        Fb = alloc("Fb", [128, 6, 384], F32)
        S.dma("sp", rb[:], dr["rel_bias"][:, :], writes=["rb"])
        memset("dve", rbT[:], 1.0, ["rbT"])
        cp("dve", rbT[0:32, :, :], rb[:].unsqueeze(2).to_broadcast([32, 6, 128]), ["rb", "rbT"], ["rbT"])
        for h in range(6):
            pb, pk = pbank()
            mm(pb[:, 0:384], rbT[:, h, :], OH, True, True, ["rbT", "cst"], [pk])
            act(Fb[:, h, :], pb[:, 0:384], AF.Copy, [pk], ["Fb"])
        S.dma("sp", zscr.rearrange("h p i -> p h i"), Fb[:], reads=["Fb"], writes=["zscr"])
        base[0] = phase_base
        S.barrier()

        for l in range(NL):
            base[0] = phase_base
            wa = alloc("wa", [128, 8, 512], F32)
            for n in range(12):
                S.dma("sp", wa[:], dr["w_ada"][l, :, n * 512:(n + 1) * 512].rearrange("(k p) n -> p k n", p=128),
                      writes=["wa"])
                S.dma("sp", rowt[0:1, 0:512], dr["b_ada"][l:l + 1, n * 512:(n + 1) * 512], writes=["rowt"])
                pb, pk = pbank()
                for kc in range(8):
                    mm(pb[0:1, :], ca[:, kc:kc + 1], wa[:, kc, :], kc == 0, False, ["ca", "wa"], [pk])
                mm(pb[0:1, :], ones[0:1, 0:1], rowt[0:1, 0:512], False, True, ["cst", "rowt"], [pk])
                act(rowm[0:1, :], pb[0:1, :], AF.Copy, [pk], ["rowm"])
                which = n // 2
                half = n % 2
                if which in (0, 1, 3, 4):
                    slot = {0: 0, 1: 1, 3: 2, 4: 3}[which]
                    pb2, pk2 = pbank()
                    for j in range(4):
                        mm(pb2[:, j:j + 1], rowm[0:1, j * 128:(j + 1) * 128], ones[0:1, 0:1], True, True,
                           ["rowm", "cst"], [pk2])
                    if slot in (1, 3):
                        ts("dve", modcol[:, slot, half * 4:half * 4 + 4], pb2[:, 0:4], 1.0, None, ALU.add, None,
                           [pk2], ["modcol"])
                    else:
                        cp("dve", modcol[:, slot, half * 4:half * 4 + 4], pb2[:, 0:4], [pk2], ["modcol"])
                else:
                    gi = 0 if which == 2 else 1
                    pb2, pk2 = pbank()
                    mm(pb2[:, :], ones[0:1, 0:128], rowm[0:1, :], True, True, ["cst", "rowm"], [pk2])
                    act(gbc[:, gi, half * 512:(half + 1) * 512], pb2[:, :], AF.Copy, [pk2], ["gbc"])
            S.barrier()

            base[0] = phase_base
            lnb = alloc("lnb", [128, 2, D], F32)
            for i, nm in enumerate(("ln1_g", "ln1_b")):
                bcast_row(lnb[:, i, :], dr[nm][l:l + 1, :], 1024, "lnb")
            BM = alloc("BM", [128, 6, 256], F32)
            S.dma("sp", BM[:], bass.AP(tensor=zscr.tensor, offset=127, ap=[[383, 128], [128 * 384, 6], [1, 256]]),
                  reads=["zscr"], writes=["BM"])
            win = alloc("win", [128, 8, IN_DIM], BF16)
            wout = alloc("wout", [128, 8, D], BF16)
            stg_mark = base[0]
            stg["bufs"] = [alloc("stgA", [128, 8, 256], F32), alloc("stgB", [128, 8, 256], F32)]
            stg_end = base[0]
            for c0 in range(0, IN_DIM, 256):
                w = min(256, IN_DIM - c0)
                load_cast(win[:, :, c0:c0 + w], dr["w_in"][l, :, c0:c0 + w], w, "win")
            woutk = []
            for c0 in range(0, D, 256):
                woutk += load_cast(wout[:, :, c0:c0 + 256], dr["w_out"][l, :, c0:c0 + 256], 256, ("wout", c0), mul=gbc[:, 0, c0:c0 + 256])
            base[0] = stg_mark
            raw = alloc("raw", [128, 9, 260], F32)
            fm = alloc("fm", [128, 9, 256], F32)
            assert base[0] >= stg_end
            poolW = alloc("poolW", [128, 2, 128], F32)
            poolWb = alloc("poolWb", [128, 2, 128], BF16)
            presb = alloc("presb", [128, 256], BF16)
            memset("dve", poolW[:], 0.0, ["poolW"])
            for gi in range(4):
                ci, hf = gi // 2, gi % 2
                S.dma("sp", poolW[hf * 64:(hf + 1) * 64, ci, hf * 64:(hf + 1) * 64], dr["pool_w"][l, gi, :, :],
                      reads=["poolW"], writes=["poolW%d" % gi])
            cp("dve", poolWb[:], poolW[:], ["poolW", "poolW0", "poolW1", "poolW2", "poolW3"], ["poolWb"])
            pscale = alloc("pscale", [128, 2], F32)
            S.dma("sp", pscale[:], dr["pool_scale"][l, :].rearrange("(c p) -> p c", p=128), writes=["pscale"], slow=True)
            convw = alloc("convw", [128, 9, 4], F32)
            for j in range(4):
                S.dma("sp", convw[:, :, j], dr["conv_w"][l, j, :].rearrange("(c p) -> p c", p=128), writes=["convw%d" % j], slow=True)
            esink = alloc("esink", [128, 6], F32)
            bcast_row(esink[:, :], dr["attn_sinks"][l:l + 1, :], 6, "esink")
            act(esink[:], esink[:], AF.Exp, ["esink"], ["esink"])
            gpar = alloc("gpar", [64, 3, 64], F32)
            bcast_row(small[:, 0:6], dr["gdn_a_log"][l:l + 1, :], 6, "small")
            act(gpar[:, 0, 0:6], small[0:64, 0:6], AF.Exp, ["small"], ["gpar0"])
            ts("dve", gpar[:, 0, 0:6], gpar[:, 0, 0:6], -1.0, None, ALU.mult, None, ["gpar0"], ["gpar0"])
            bcast_row(small[:, 8:14], dr["gdn_dt_bias"][l:l + 1, :], 6, "small2")
            cp("dve", gpar[:, 1, 0:6], small[0:64, 8:14], ["small2"], ["gpar1"])
            bcast_row(small[:, 64:128], dr["gdn_norm_w"][l:l + 1, :], 64, "small3")
            cp("dve", gpar[:, 2, :], small[0:64, 64:128], ["small3"], ["gpar2"])

            xg = alloc("xg", [128, 2, D], F32)
            hT = alloc("hT", [128, 8, 256], BF16)
            yT = alloc("yT", [128, 8, 256], BF16)
            ubuf = alloc("ubuf", [128, 2, 272], F32)
            qTa = alloc("qTa", [64, 6, 256], BF16)
            kTa = alloc("kTa", [64, 2, 384], BF16)
            Vaug = alloc("Vaug", [128, 3, 2, 65], BF16)
            sT = alloc("sT", [128, 256], F32)
            eT = alloc("eT", [128, 256], BF16)
            onrm = alloc("onrm", [128, 384], F32)
            rden = alloc("rden", [128, 6], F32)
            cacc = alloc("cacc", [128, 256], F32)
            ztm = alloc("ztm", [64, 4, 384], F32)
            batm = alloc("batm", [64, 4, 12], F32)
            zz_off = base[0]
            zzb = alloc("zz", [128, 1088], F32)
            zz = zzb[:, 0:D]
            if ("ptmp", zz_off) not in acache:
                acache[("ptmp", zz_off)] = nc.alloc_sbuf_tensor_at("ptmp", [128, 4, 272], F32, offset=zz_off)
            ptmp = acache[("ptmp", zz_off)]
            S.alias["ptmp"] = "zz"
            stats = alloc("stats", [128, 2, 6], F32)
            mv = alloc("mv", [128, 2], F32)
            G = {nm: alloc("g_" + nm, [64, 12, 64], F32) for nm in
                 ("q", "k", "v", "kb", "kbg", "qd", "kd", "kT", "kbT", "qT", "qdT", "DT", "A", "N", "P", "attT", "sq")}
            for al, cn in (("A2", "q"), ("N2", "k"), ("u", "kb"), ("wT", "kT"), ("vn", "kbT"), ("o", "qd")):
                G[al] = G[cn]
                S.alias["g_" + al] = "g_" + cn
            Sst = alloc("Sst", [64, 6, 64], F32)
            gsm = alloc("gsm", [64, 12, 12], F32)
            S.barrier()
            memset("dve", Sst[:], 0.0, ["Sst"])
            memset("dve", ubuf[:], 0.0, ["ubuf"])
            memset("dve", raw[:], 0.0, ["raw"])
            memset("dve", kTa[:], 0.0, ["kTa"])
            memset("dve", Vaug[:], 0.0, ["Vaug"])
            memset("dve", Vaug[:, :, :, 64:65], 1.0, ["Vaug"])

            C_POOL, C_Q, C_K, C_V, C_G, C_Z, C_B = 0, 256, 640, 768, 896, 2048, 2432

            xsrc = dr["x"] if l == 0 else xs
            for g in range(NG):
                t0 = 2 * g
                for i in range(2):
                    S.dma("sp", xg[:, i, :], xsrc[(t0 + i) * 128:(t0 + i + 1) * 128, :], reads=[("xs", t0 + i)], writes=[("xg", i)])
                for kc in range(8):
                    if kc % 2 == 0:
                        pb, pk = pbank()
                    for i in range(2):
                        tr(pb[:, (kc % 2) * 256 + i * 128:(kc % 2) * 256 + (i + 1) * 128],
                           xg[:, i, kc * 128:(kc + 1) * 128], ident, [("xg", i), "cst"], [pk])
                    act(hT[:, kc, :], pb[:, (kc % 2) * 256:(kc % 2 + 1) * 256], AF.Identity, [pk, "modcol"], ["hT"],
                        scale=modcol[:, 1, kc:kc + 1], bias=modcol[:, 0, kc:kc + 1])

                def proj_fm(col0, M, evac):
                    pb, pk = pbank()
                    for kc in range(8):
                        mm(pb[0:M, 0:256], win[:, kc, col0:col0 + M], hT[:, kc, :], kc == 0, kc == 7, ["win", "hT"], [pk])
                    evac(pb, pk)

                for ci in range(2):
                    proj_fm(C_POOL + ci * 128, 128,
                            lambda pb, pk, ci=ci: act(ubuf[:, ci, 16:272], pb[:, 0:256], AF.Copy, [pk], ["ubuf"]))
                for h in range(6):
                    proj_fm(C_Q + h * 64, 64,
                            lambda pb, pk, h=h: act(qTa[:, h, :], pb[0:64, 0:256], AF.Copy, [pk], ["qTa"]))
                for h in range(2):
                    proj_fm(C_K + h * 64, 64,
                            lambda pb, pk, h=h: act(kTa[:, h, 128:384], pb[0:64, 0:256], AF.Copy, [pk], ["kTa"]))
                for ci in range(9):
                    proj_fm(C_G + ci * 128, 128,
                            lambda pb, pk, ci=ci: cp("dve", raw[:, ci, 4:260], pb[:, 0:256], [pk], ["raw"]))
                for i in range(2):
                    pb, pk = pbank()
                    for kc in range(8):
                        mm(pb[:, 0:128], hT[:, kc, i * 128:(i + 1) * 128], win[:, kc, C_V:C_V + 128], kc == 0, kc == 7,
                           ["win", "hT"], [pk])
                    act(Vaug[:, 1 + i, :, 0:64], pb[:, 0:128].rearrange("p (g d) -> p g d", g=2), AF.Copy, [pk], ["Vaug"])
                for cch in range(4):
                    pb, pk = pbank()
                    for kc in range(8):
                        mm(pb[0:64, 0:396], hT[:, kc, cch * 64:(cch + 1) * 64], win[:, kc, C_Z:C_Z + 396], kc == 0, kc == 7,
                           ["win", "hT"], [pk])
                    act(ztm[:, cch, :], pb[0:64, 0:384], AF.Silu, [pk], ["ztm"])
                    cp("dve", batm[:, cch, :], pb[0:64, 384:396], [pk], ["batm"])

                for ci in (range(2) if 'p' not in KSKIP else ()):
                    u_ = ubuf[:, ci, :]
                    s2, s4, s8, s16 = (ptmp[:, j, :] for j in range(4))
                    tt("dve", s2[:, 1:272], u_[:, 1:272], u_[:, 0:271], ALU.add, ["ubuf"], ["ptmp"])
                    tt("dve", s4[:, 3:272], s2[:, 3:272], s2[:, 1:270], ALU.add, ["ptmp"], ["ptmp"])
                    if ci == 1:
                        tt("dve", s8[:, 7:272], s4[:, 7:272], s4[:, 3:268], ALU.add, ["ptmp"], ["ptmp"])
                        tt("dve", s16[:, 15:272], s8[:, 15:272], s8[:, 7:264], ALU.add, ["ptmp"], ["ptmp"])
                        lo, hi, wl, wh = s8, s16, 8.0, 16.0
                    else:
                        lo, hi, wl, wh = s2, s4, 2.0, 4.0
                    pres = presb
                    if g == 0:
                        tt("dve", lo[0:64, 16:32], lo[0:64, 16:32], cst[0:64, CW_PF + ci * 16:CW_PF + ci * 16 + 16],
                           ALU.mult, ["ptmp", "cst"], ["ptmp"])
                        tt("dve", hi[64:128, 16:32], hi[64:128, 16:32],
                           cst[64:128, CW_PF + ci * 16:CW_PF + ci * 16 + 16], ALU.mult, ["ptmp", "cst"], ["ptmp"])
                    stt("dve", pres[0:64, :], lo[0:64, 16:272], 1.0 / wl, u_[0:64, 16:272], ALU.mult, ALU.subtract,
                        ["ptmp", "ubuf"], ["presb"])
                    stt("dve", pres[64:128, :], hi[64:128, 16:272], 1.0 / wh, u_[64:128, 16:272], ALU.mult, ALU.subtract,
                        ["ptmp", "ubuf"], ["presb"])
                    if 'x' not in KSKIP:
                        pb, pk = pbank()
                        mm(pb[:, 0:256], poolWb[:, ci, :], pres[:, :], True, True, ["poolWb", "presb"], [pk])
                        if 'w' not in KSKIP:
                            act(yT[:, ci, :], pb[:, 0:256], AF.Identity, [pk, "pscale"], ["yT"], scale=pscale[:, ci:ci + 1])
                        else:
                            act(yT[:, ci, :], pb[:, 0:256], AF.Copy, [pk, "pscale"], ["yT"])
                    cp("dve", ubuf[:, ci, 0:16], ubuf[:, ci, 256:272], ["ubuf", "presb"], ["ubuf"])

                for i in (range(2) if 'a' not in KSKIP else ()):
                    pbo, pko = pf[7], ("pf", 7)
                    for h in range(6):
                        gk = h // 3
                        pb, pk = pbank()
                        first = (g == 0 and i == 0)
                        mm(pb[:, 0:128], kTa[:, gk, 128 + i * 128:256 + i * 128], qTa[:, h, i * 128:(i + 1) * 128],
                           True, True, ["kTa", "qTa"], [pk])
                        if not first:
                            mm(pb[:, 128:256], kTa[:, gk, i * 128:128 + i * 128], qTa[:, h, i * 128:(i + 1) * 128],
                               True, True, ["kTa", "qTa"], [pk])
                        ncol = 128 if first else 256
                        stt("dve", sT[:, 0:ncol], pb[:, 0:ncol], 0.125, BM[:, h, 0:ncol], ALU.mult, ALU.add,
                            [pk, "BM"], ["sT"])
                        act(eT[:, 0:ncol], sT[:, 0:ncol], AF.Exp, ["sT"], ["eT"])
                        mm(pbo[:, h * 65:(h + 1) * 65], eT[:, 0:128], Vaug[:, 1 + i, gk, :], True, first,
                           ["eT", "Vaug"], [pko])
                        if not first:
                            mm(pbo[:, h * 65:(h + 1) * 65], eT[:, 128:256], Vaug[:, i, gk, :], False, True,
                               ["eT", "Vaug"], [pko])
                    po3 = pbo[:, 0:390].rearrange("p (h e) -> p h e", h=6)
                    tt("dve", rden[:], po3[:, :, 64], esink[:], ALU.add, [pko, "esink"], ["rden"])
                    S.op("dve", lambda e: e.reciprocal(out=rden[:], in_=rden[:]), ["rden"], ["rden"])
                    tt("dve", onrm[:].rearrange("p (h d) -> p h d", h=6), po3[:, :, 0:64],
                       rden[:].unsqueeze(2).to_broadcast([128, 6, 64]), ALU.mult, [pko, "rden"], ["onrm"])
                    if dbg is not None and g == 0 and i == 0:
                        S.dma("sp", dbg[:, 0:384], onrm[:], reads=["onrm"])
                        S.dma("sp", dbg[:, 384:390], rden[:], reads=["rden"])
                        S.dma("sp", dbg[:, 400:528], sT[:, 0:128], reads=["sT"])
                    hb, hk = pbank()
                    for c3 in range(3):
                        tr(hb[:, c3 * 128:(c3 + 1) * 128], onrm[:, c3 * 128:(c3 + 1) * 128], ident, ["onrm", "cst"], [hk])
                    act(yT[:, 2:5, i * 128:(i + 1) * 128], hb[:, 0:384].rearrange("p (c t) -> p c t", c=3), AF.Copy,
                        [hk], ["yT"])
                cp("dve", kTa[:, :, 0:128], kTa[:, :, 256:384], ["kTa"], ["kTa"])
                cp("dve", Vaug[:, 0, :, :], Vaug[:, 2, :, :], ["Vaug"], ["Vaug"])

                for ci in (range(9) if 'c' not in KSKIP else ()):
                    ts("dve", cacc[:], raw[:, ci, 4:260], convw[:, ci, 3:4], None, ALU.mult, None, ["raw", "convw0", "convw1", "convw2", "convw3"], ["cacc"])
                    for j in range(3):
                        stt("dve", cacc[:], raw[:, ci, 1 + j:257 + j], convw[:, ci, j:j + 1], cacc[:], ALU.mult, ALU.add,
                            ["raw", "convw0", "convw1", "convw2", "convw3", "cacc"], ["cacc"])
                    act(fm[:, ci, :], cacc[:], AF.Silu, ["cacc"], ["fm"])
                cp("dve", raw[:, :, 0:4], raw[:, :, 256:260], ["raw", "fm"], ["raw"])

                for cp2 in (range(2) if 'g' not in KSKIP else ()):
                    W = 768

                    def fl(nm):
                        return G[nm][:].rearrange("p m d -> p (m d)")
                    for qi, nm in enumerate(("q", "k", "v")):
                        pb, pk = pbank2()
                        for c2 in range(2):
                            tok0 = (cp2 * 2 + c2) * 64
                            for c3 in range(3):
                                tr(pb[0:64, c2 * 384 + c3 * 128:c2 * 384 + (c3 + 1) * 128], fm[:, qi * 3 + c3, tok0:tok0 + 64],
                                   ident, ["fm", "cst"], pk)
                        act(fl(nm), pb[0:64, 0:W], AF.Copy, pk, ["g_" + nm])
                    for nm, col, scl in (("q", 0, 0.125), ("k", 1, 1.0)):
                        gk_ = "gsm%d" % col
                        tt("dve", G["sq"][:], G[nm][:], G[nm][:], ALU.mult, ["g_" + nm], ["g_sq"])
                        S.op("dve", lambda e, col=col: e.tensor_reduce(out=gsm[:, col, :], in_=G["sq"][:], axis=AX.X,
                                                                       op=ALU.add), ["g_sq"], [gk_])
                        act(gsm[:, col, :], gsm[:, col, :], AF.Sqrt, [gk_], [gk_], bias=1e-6)
                        S.op("dve", lambda e, col=col: e.reciprocal(out=gsm[:, col, :], in_=gsm[:, col, :]), [gk_], [gk_])
                        if scl != 1.0:
                            ts("dve", gsm[:, col, :], gsm[:, col, :], scl, None, ALU.mult, None, [gk_], [gk_])
                        tt("dve", G[nm][:], G[nm][:], gsm[:, col, :].unsqueeze(2).to_broadcast([64, 12, 64]), ALU.mult,
                           ["g_" + nm, gk_], ["g_" + nm])
                    b3 = batm[:, cp2 * 2:cp2 * 2 + 2, :]
                    g2v = lambda col: gsm[:, col, :].rearrange("p (c h) -> p c h", c=2)
                    act(g2v(2), b3[:, :, 0:6], AF.Sigmoid, ["batm"], ["gsm2"])
                    tt("dve", g2v(3), b3[:, :, 6:12], gpar[:, 1, 0:6].unsqueeze(1).to_broadcast([64, 2, 6]), ALU.add,
                       ["batm", "gpar1"], ["gsm3"])
                    act(gsm[:, 3, :], gsm[:, 3, :], AF.Exp, ["gsm3"], ["gsm3"])
                    act(gsm[:, 3, :], gsm[:, 3, :], AF.Ln, ["gsm3"], ["gsm3"], bias=1.0)
                    tt("dve", g2v(3), g2v(3), gpar[:, 0, 0:6].unsqueeze(1).to_broadcast([64, 2, 6]), ALU.mult,
                       ["gsm3", "gpar0"], ["gsm3"])
                    pb, pk = pbank()
                    mm(pb[0:64, 0:12], U64, gsm[:, 3, :], True, True, ["cst", "gsm3"], [pk])
                    mm(pb[0:64, 16:28], ones[0:64, 0:64], gsm[:, 3, :], True, True, ["cst", "gsm3"], [pk])
                    cp("dve", gsm[:, 4, :], pb[0:64, 0:12], [pk], ["gsm4"])
                    cp("dve", gsm[:, 5, :], pb[0:64, 16:28], [pk], ["gsm5"])
                    act(gsm[:, 6, :], gsm[:, 4, :], AF.Exp, ["gsm4"], ["gsm6"])
                    tt("dve", gsm[:, 7, :], gsm[:, 5, :], gsm[:, 4, :], ALU.subtract, ["gsm4", "gsm5"], ["gsm7"])
                    act(gsm[:, 7, :], gsm[:, 7, :], AF.Exp, ["gsm7"], ["gsm7"])
                    act(gsm[:, 8, :], gsm[:, 5, :], AF.Exp, ["gsm5"], ["gsm8"])

                    def bc(col):
                        return gsm[:, col, :].unsqueeze(2).to_broadcast([64, 12, 64])
                    tt("dve", G["kb"][:], G["k"][:], bc(2), ALU.mult, ["g_k", "gsm2"], ["g_kb"])
                    tt("pool", G["v"][:], G["v"][:], bc(2), ALU.mult, ["g_v", "gsm2"], ["g_v"])
                    tt("dve", G["kbg"][:], G["kb"][:], bc(6), ALU.mult, ["g_kb", "gsm6"], ["g_kbg"])
                    tt("pool", G["qd"][:], G["q"][:], bc(6), ALU.mult, ["g_q", "gsm6"], ["g_qd"])
                    tt("pool", G["kd"][:], G["k"][:], bc(7), ALU.mult, ["g_k", "gsm7"], ["g_kd"])
                    for src, dstn in (("k", "kT"), ("kb", "kbT"), ("q", "qT"), ("qd", "qdT")):
                        pb, pk = pbank2()
                        for m in range(12):
                            tr(pb[0:64, m * 64:(m + 1) * 64], G[src][:, m, :], ident[0:64, 0:64], ["g_" + src, "cst"], pk)
                        act(fl(dstn), pb[0:64, 0:W], AF.Copy, pk, ["g_" + dstn])
                    I12 = cst[0:64, CW_I8:CW_I8 + 64].unsqueeze(1).to_broadcast([64, 12, 64])
                    tt("dve", G["sq"][:], I12, bc(4), ALU.mult, ["cst", "gsm4"], ["g_sq"])
                    ts("pool", G["DT"][:], bc(4), -1.0, None, ALU.mult, None, ["gsm4"], ["g_DT"])
                    pb, pk = pbank2()
                    for hb_ in range(3):
                        cs = slice(hb_ * 256, (hb_ + 1) * 256)
                        mm(pb[0:64, cs], ones[0:64, 0:64], fl("sq")[:, cs], True, False, ["cst", "g_sq"], pk)
                        mm(pb[0:64, cs], ident[0:64, 0:64], fl("DT")[:, cs], False, False, ["cst", "g_DT"], pk)
                        mm(pb[0:64, cs], ident[0:64, 0:64], MB8[:, 0:256], False, True, ["cst"], pk)
                    act(fl("DT"), pb[0:64, 0:W], AF.Exp, pk, ["g_DT"])
                    pb, pk = pbank2()
                    pb2, pk2 = pbank2()
                    for m in range(12):
                        mm(pb[0:64, m * 64:(m + 1) * 64], G["kT"][:, m, :], G["kbT"][:, m, :], True, True,
                           ["g_kT", "g_kbT"], pk)
                        mm(pb2[0:64, m * 64:(m + 1) * 64], G["kT"][:, m, :], G["qT"][:, m, :], True, True,
                           ["g_kT", "g_qT"], pk2)
                    NS12 = cst[0:64, CW_NS:CW_NS + 64].unsqueeze(1).to_broadcast([64, 12, 64])
                    tt("dve", fl("A"), pb[0:64, 0:W], fl("DT"), ALU.mult, pk + ["g_DT"], ["g_A"])
                    tt("pool", G["A"][:], G["A"][:], NS12, ALU.mult, ["g_A", "cst"], ["g_A"])
                    tt("dve", fl("attT"), pb2[0:64, 0:W], fl("DT"), ALU.mult, pk2 + ["g_DT"], ["g_attT"])
                    pb, pk = pbank2()
                    for m in range(12):
                        tr(pb[0:64, m * 64:(m + 1) * 64], G["A"][:, m, :], ident[0:64, 0:64], ["g_A", "cst"], pk)
                    act(fl("N"), pb[0:64, 0:W], AF.Copy, pk, ["g_N"])
                    tt("pool", G["P"][:], G["A"][:], I12, ALU.add, ["g_A", "cst"], ["g_P"])
                    An, Nn, Ao, No = "A", "N", "A2", "N2"
                    for lev in range(1, 6):
                        pbn, pkn = pbank2()
                        for m in range(12):
                            mm(pbn[0:64, m * 64:(m + 1) * 64], G[An][:, m, :], G[Nn][:, m, :], True, True,
                               ["g_" + An, "g_" + Nn], pkn)
                        act(fl(No), pbn[0:64, 0:W], AF.Copy, pkn, ["g_" + No])
                        if lev < 5:
                            pba, pka = pbank2()
                            for m in range(12):
                                mm(pba[0:64, m * 64:(m + 1) * 64], G[Nn][:, m, :], G[An][:, m, :], True, True,
                                   ["g_" + An, "g_" + Nn], pka)
                            cp("dve", fl(Ao), pba[0:64, 0:W], pka, ["g_" + Ao])
                        pbp, pkp = pbank2()
                        for m in range(12):
                            mm(pbp[0:64, m * 64:(m + 1) * 64], G[No][:, m, :], G["P"][:, m, :], True, True,
                               ["g_" + No, "g_P"], pkp)
                        tt("dve", fl("P"), fl("P"), pbp[0:64, 0:W], ALU.add, ["g_P"] + pkp, ["g_P"])
                        An, Ao = Ao, An
                        Nn, No = No, Nn
                    pb, pk = pbank2()
                    pb2, pk2 = pbank2()
                    for m in range(12):
                        mm(pb[0:64, m * 64:(m + 1) * 64], G["P"][:, m, :], G["v"][:, m, :], True, True, ["g_P", "g_v"], pk)
                        mm(pb2[0:64, m * 64:(m + 1) * 64], G["kbg"][:, m, :], G["P"][:, m, :], True, True,
                           ["g_P", "g_kbg"], pk2)
                    act(fl("u"), pb[0:64, 0:W], AF.Copy, pk, ["g_u"])
                    cp("dve", fl("wT"), pb2[0:64, 0:W], pk2, ["g_wT"])
                    for c2 in range(2):
                        cch = cp2 * 2 + c2
                        tok0 = cch * 64
                        ms = slice(c2 * 6, c2 * 6 + 6)
                        cs = slice(c2 * 384, (c2 + 1) * 384)
                        pb, pk = pbank()
                        for h in range(6):
                            mm(pb[0:64, h * 64:(h + 1) * 64], G["wT"][:, c2 * 6 + h, :], Sst[:, h, :], True, True,
                               ["g_wT", "Sst"], [pk])
                        tt("dve", fl("vn")[:, cs], fl("u")[:, cs], pb[0:64, 0:384], ALU.subtract, ["g_u", pk], [("g_vn", c2)])
                        pbo, pko = pbank()
                        pbs, pks = pbank()
                        for h in range(6):
                            m = c2 * 6 + h
                            mm(pbo[0:64, h * 64:(h + 1) * 64], G["qdT"][:, m, :], Sst[:, h, :], True, False, ["g_qdT", "Sst"], [pko])
                            mm(pbo[0:64, h * 64:(h + 1) * 64], G["attT"][:, m, :], G["vn"][:, m, :], False, True,
                               ["g_attT", ("g_vn", c2)], [pko])
                            mm(pbs[0:64, h * 64:(h + 1) * 64], G["kd"][:, m, :], G["vn"][:, m, :], True, True,
                               ["g_kd", ("g_vn", c2)], [pks])
                        tt("dve", Sst[:], Sst[:], gsm[:, 8, c2 * 6:c2 * 6 + 6].unsqueeze(2).to_broadcast([64, 6, 64]), ALU.mult,
                           ["Sst", "gsm8"], ["Sst"])
                        tt("dve", Sst[:].rearrange("p h d -> p (h d)"), Sst[:].rearrange("p h d -> p (h d)"), pbs[0:64, 0:384],
                           ALU.add, ["Sst", pks], ["Sst"])
                        act(fl("o")[:, cs], pbo[0:64, 0:384], AF.Copy, [pko], [("g_o", c2)])
                    tt("pool", G["sq"][:], G["o"][:], G["o"][:], ALU.mult, [("g_o", 0), ("g_o", 1)], ["g_sq"])
                    S.op("dve", lambda e: e.tensor_reduce(out=gsm[:, 9, :], in_=G["sq"][:], axis=AX.X, op=ALU.add),
                         ["g_sq"], ["gsm9"])
                    act(gsm[:, 9, :], gsm[:, 9, :], AF.Sqrt, ["gsm9"], ["gsm9"], scale=1.0 / 64, bias=1e-6)
                    S.op("dve", lambda e: e.reciprocal(out=gsm[:, 9, :], in_=gsm[:, 9, :]), ["gsm9"], ["gsm9"])
                    tt("dve", G["o"][:], G["o"][:], bc(9), ALU.mult, [("g_o", 0), ("g_o", 1), "gsm9"], [("g_o", 0), ("g_o", 1)])
                    tt("pool", G["o"][:], G["o"][:], gpar[:, 2, :].unsqueeze(1).to_broadcast([64, 12, 64]), ALU.mult,
                       [("g_o", 0), ("g_o", 1), "gpar2"], [("g_o", 0), ("g_o", 1)])
                    tt("dve", fl("sq"), fl("o"), ztm[:, cp2 * 2:cp2 * 2 + 2, :].rearrange("p c f -> p (c f)"), ALU.mult,
                       [("g_o", 0), ("g_o", 1), "ztm"], ["g_sq"])
                    pb, pk = pbank()
                    for c2 in range(2):
                        for c3 in range(3):
                            tr(pb[:, (c3 * 2 + c2) * 64:(c3 * 2 + c2 + 1) * 64],
                               fl("sq")[:, c2 * 384 + c3 * 128:c2 * 384 + (c3 + 1) * 128], ident[0:64, 0:64], ["g_sq", "cst"], [pk])
                    act(yT[:, 5:8, cp2 * 128:(cp2 + 1) * 128], pb[:, 0:384].rearrange("p (c t) -> p c t", c=3), AF.Copy, [pk], ["yT"])

                for mi, (a0, a1) in enumerate(((0, 2), (2, 5), (5, 8))):
                    if not ymask[mi]:
                        memset("dve", yT[:, a0:a1, :], 0.0, ["yT"])
                for i in range(2):
                    t = t0 + i
                    for hf in range(2):
                        pb, pk = pbank()
                        for kc in range(8):
                            mm(pb[:, :], yT[:, kc, i * 128:(i + 1) * 128], wout[:, kc, hf * 512:(hf + 1) * 512], kc == 0,
                               kc == 7, ["yT"] + woutk, [pk])
                        stt("dve", zz[:, hf * 512:(hf + 1) * 512], xg[:, i, hf * 512:(hf + 1) * 512], ALPHA, pb[:, :],
                            ALU.mult, ALU.add, [("xg", i), pk], ["zz"])
                    layer_norm_tile(S, zz, xg[:, i, :], lnb[:, 0, :], lnb[:, 1, :], stats, mv, ("xg", i))
                    S.dma("sp", xs[t * 128:(t + 1) * 128, :], xg[:, i, :], reads=[("xg", i)], writes=[("xs", t)])
            S.barrier()

            if not do_moe:
                continue
            base[0] = phase_base
            xres = alloc("xres", [128, NT, D], F32)
            for t in range(NT):
                S.dma("sp", xres[:, t, :], xs[t * 128:(t + 1) * 128, :], reads=[("xs", t)], writes=[("x", t)])
            h2T = alloc("h2T", [128, 8, T], BF16)
            Gt = alloc("Gt", [128, NT, NE], F32)
            stats2 = alloc("stats2", [128, 2, 6], F32)
            mv2 = alloc("mv2", [128, 2], F32)
            moe_mark = base[0]
            h32 = alloc("h32", [128, 8, 128], F32)
            rw = alloc("rw", [128, 8, NE], F32)
            lg = alloc("lg", [128, NE], F32)
            mx8 = alloc("mx8", [128, 8], F32)
            rbb = alloc("rbb", [128, NE], F32)
            gsum = alloc("gsum", [128, 1], F32)
            bdn = alloc("bdn", [NE, D], F32)
            bdnb = alloc("bdnb", [NE, D], BF16)
            GTb = alloc("GTb", [NE, T], BF16)

            S.dma("sp", rw[:], dr["router_w"][l, :, :].rearrange("(k p) n -> p k n", p=128), writes=["rw"])
            bcast_row(rbb[:, :], dr["router_b"][l:l + 1, :], NE, "rbb")
            S.dma("sp", bdn[:], dr["exp_b_down"][l, :, :], writes=["bdn"])
            tt("dve", bdnb[:], bdn[:], gbc[0:NE, 1, :], ALU.mult, ["bdn", "gbc"], ["bdnb"])
            for t in range(NT):
                for kc in range(8):
                    if kc % 4 == 0:
                        pb, pk = pbank()
                    tr(pb[:, (kc % 4) * 128:(kc % 4 + 1) * 128], xres[:, t, kc * 128:(kc + 1) * 128], ident,
                       [("x", t), "cst"], [pk])
                    act(h2T[:, kc, t * 128:(t + 1) * 128], pb[:, (kc % 4) * 128:(kc % 4 + 1) * 128], AF.Identity,
                        [pk, "modcol"], ["h2T"], scale=modcol[:, 3, kc:kc + 1], bias=modcol[:, 2, kc:kc + 1])
                    act(h32[:, kc, :], pb[:, (kc % 4) * 128:(kc % 4 + 1) * 128], AF.Identity,
                        [pk, "modcol"], ["h32"], scale=modcol[:, 3, kc:kc + 1], bias=modcol[:, 2, kc:kc + 1])
                pb, pk = pbank()
                for kc in range(8):
                    mm(pb[:, 0:NE], h32[:, kc, :], rw[:, kc, :], kc == 0, kc == 7, ["h32", "rw"], [pk])
                tt("dve", lg[:], pb[:, 0:NE], rbb[:], ALU.add, [pk, "rbb"], ["lg"])
                S.op("dve", lambda e: e.max(out=mx8[:], in_=lg[:]), ["lg"], ["mx8"])
                ts("dve", Gt[:, t, :], lg[:], mx8[:, 3:4], None, ALU.is_ge, None, ["lg", "mx8"], [("Gt", t)])
                ts("dve", lg[:], lg[:], mx8[:, 0:1], None, ALU.subtract, None, ["lg", "mx8"], ["lg"])
                act(lg[:], lg[:], AF.Exp, ["lg"], ["lg"])
                tt("dve", Gt[:, t, :], Gt[:, t, :], lg[:], ALU.mult, [("Gt", t), "lg"], [("Gt", t)])
                S.op("dve", lambda e, t=t: e.tensor_reduce(out=gsum[:], in_=Gt[:, t, :], axis=AX.X, op=ALU.add),
                     [("Gt", t)], ["gsum"])
                S.op("dve", lambda e: e.reciprocal(out=gsum[:], in_=gsum[:]), ["gsum"], ["gsum"])
                ts("dve", Gt[:, t, :], Gt[:, t, :], gsum[:, 0:1], None, ALU.mult, None, [("Gt", t), "gsum"], [("Gt", t)])
                pb, pk = pbank()
                tr(pb[0:NE, 0:128], Gt[:, t, :], ident, [("Gt", t), "cst"], [pk])
                act(GTb[:, t * 128:(t + 1) * 128], pb[0:NE, 0:128], AF.Copy, [pk], ["GTb"])
                for hf in range(2):
                    pb, pk = pbank()
                    mm(pb[:, :], GTb[:, t * 128:(t + 1) * 128], bdnb[:, hf * 512:(hf + 1) * 512], True, True,
                       ["GTb", "bdnb"], [pk])
                    stt("dve", xres[:, t, hf * 512:(hf + 1) * 512], xres[:, t, hf * 512:(hf + 1) * 512], ALPHA, pb[:, :],
                        ALU.mult, ALU.add, [("x", t), pk, "h2T", "h32"], [("x", t)])
            S.barrier()
            base[0] = moe_mark
            NUPR = 2
            wupr = [alloc("wupr%d" % i, [128, 8, 256], BF16) for i in range(NUPR)]
            wdnr = [alloc("wdnr%d" % i, [128, 8, 512], BF16) for i in range(2)]
            bup = alloc("bup", [128, 2, 16], F32)
            glb = [alloc("gl%d" % i, [128, 512], F32) for i in range(2)]
            sgb = [alloc("sg%d" % i, [128, 512], F32) for i in range(2)]
            llb = [alloc("ll%d" % i, [128, 512], F32) for i in range(2)]
            actT = alloc("actT", [128, 8, T], BF16)
            stg["bufs"] = [alloc("stgA", [128, 8, 256], F32), alloc("stgB", [128, 8, 256], F32)]
            ts("dve", gbc[:, 1, :], gbc[:, 1, :], 1.0 / 1.702, None, ALU.mult, None, ["gbc", "bdnb"], ["gbc"])
            NTG = T // 512
            ui = 0
            di = 0
            gi_ = 0
            for e in range(n_exp):
                bslot = e % 2
                S.dma("sp", bup[:, bslot, :], dr["exp_b_up"][l, e, :].rearrange("(c p) -> p c", p=128),
                      writes=[("bup", bslot)], slow=True)
                ts("pool", bup[:, bslot, 8:16], bup[:, bslot, 8:16], 1.0, None, ALU.add, None, [("bup", bslot)], [("bup", bslot)])
                pending = None
                for fc in range(8):
                    wu = wupr[ui % NUPR]
                    wk = "wupr%d" % (ui % NUPR)
                    ui += 1
                    wks = load_cast(wu[:, :, :], None, 256, wk, srcs=[
                        (0, 128, dr["exp_w_up"][l, e, :, fc * 128:(fc + 1) * 128]),
                        (128, 128, dr["exp_w_up"][l, e, :, D + fc * 128:D + (fc + 1) * 128])])
                    for tg in range(NTG):
                        b2 = gi_ % 2
                        gi_ += 1
                        gl, sg, ll = glb[b2], sgb[b2], llb[b2]
                        pbg, pkg = pbank()
                        pbl, pkl = pbank()
                        for kc in range(8):
                            mm(pbg[:, :], wu[:, kc, 0:128], h2T[:, kc, tg * 512:(tg + 1) * 512], kc == 0, kc == 7,
                               wks + ["h2T"], [pkg])
                        for kc in range(8):
                            mm(pbl[:, :], wu[:, kc, 128:256], h2T[:, kc, tg * 512:(tg + 1) * 512], kc == 0, kc == 7,
                               wks + ["h2T"], [pkl])
                        ts("dve", gl[:], pbg[:, :], bup[:, bslot, fc:fc + 1], 7.0, ALU.add, ALU.min,
                           [pkg, ("bup", bslot)], ["gl%d" % b2])
                        ts("dve", ll[:], pbl[:, :], bup[:, bslot, 8 + fc:9 + fc], 8.0, ALU.add, ALU.min,
                           [pkl, ("bup", bslot)], ["ll%d" % b2])
                        act(sg[:], gl[:], AF.Silu, ["gl%d" % b2], ["sg%d" % b2], scale=1.702)
                        if pending is not None:
                            pfc, ptg, pb2 = pending
                            stt("dve", actT[:, pfc, ptg * 512:(ptg + 1) * 512], llb[pb2][:], -6.0, sgb[pb2][:], ALU.max, ALU.mult,
                                ["ll%d" % pb2, "sg%d" % pb2], [("actT", ptg)])
                        pending = (fc, tg, b2)
                pfc, ptg, pb2 = pending
                stt("dve", actT[:, pfc, ptg * 512:(ptg + 1) * 512], llb[pb2][:], -6.0, sgb[pb2][:], ALU.max, ALU.mult,
                    ["ll%d" % pb2, "sg%d" % pb2], [("actT", ptg)])
                for hf in range(2):
                    wd = wdnr[di % 2]
                    dk = "wdnr%d" % (di % 2)
                    di += 1
                    dks = []
                    for c0 in range(0, 512, 256):
                        dks += load_cast(wd[:, :, c0:c0 + 256], dr["exp_w_down"][l, e, :, hf * 512 + c0:hf * 512 + c0 + 256], 256,
                                         (dk, c0), mul=gbc[:, 1, hf * 512 + c0:hf * 512 + c0 + 256])
                    for t in range(NT):
                        pb, pk = pbank()
                        for fc in range(8):
                            mm(pb[:, :], actT[:, fc, t * 128:(t + 1) * 128], wd[:, fc, :], fc == 0, fc == 7,
                               [("actT", t // 4)] + dks, [pk])
                        stt("dve", xres[:, t, hf * 512:(hf + 1) * 512], pb[:, :], Gt[:, t, e:e + 1],
                            xres[:, t, hf * 512:(hf + 1) * 512], ALU.mult, ALU.add, [pk, ("Gt", t), ("x", t)], [("x", t)])
            S.barrier()
            base[0] = moe_mark
            lnb = alloc("lnb2", [128, 2, D], F32)
            for i, nm in enumerate(("ln2_g", "ln2_b")):
                bcast_row(lnb[:, i, :], dr[nm][l:l + 1, :], 1024, "lnb")
            dst = out if l == NL - 1 else xs
            for t in range(NT):
                layer_norm_tile(S, xres[:, t, :], xres[:, t, :], lnb[:, 0, :], lnb[:, 1, :], stats2, mv2, ("x", t), key=("x", t))
                S.dma("sp", dst[t * 128:(t + 1) * 128, :], xres[:, t, :], reads=[("x", t)], writes=[("xs", t)])
            S.barrier()
        if not do_moe:
            base[0] = phase_base
            xg2 = alloc("xg2", [128, D], F32)
            for t in range(NT):
                S.dma("sp", xg2[:], xs[t * 128:(t + 1) * 128, :], reads=[("xs", t)], writes=["xg2"])
                S.dma("sp", out[t * 128:(t + 1) * 128, :], xg2[:], reads=["xg2"])
        S.emit()
    return nc


def layer_norm_tile(S, zz, dst, gam, bet, stats, mv, dkey, key="zz"):
    sk = "st_" + str(key)
    mk = "mv_" + str(key)
    for hf in range(2):
        S.op("dve", lambda e, hf=hf: e.bn_stats(out=stats[:, hf, :], in_=zz[:, hf * 512:(hf + 1) * 512]), [key], [sk + str(hf)])
    S.op("dve", lambda e: e.bn_aggr(out=mv[:], in_=stats[:].rearrange("p a b -> p (a b)")), [sk + "0", sk + "1"], [mk])
    S.op("act", lambda e: e.activation(out=mv[:, 1:2], in_=mv[:, 1:2], func=AF.Sqrt, bias=1e-5), [mk], [mk])
    S.op("dve", lambda e: e.reciprocal(out=mv[:, 1:2], in_=mv[:, 1:2]), [mk], [mk])
    S.op("dve", lambda e: e.tensor_scalar(out=zz[:], in0=zz[:], scalar1=mv[:, 0:1], scalar2=mv[:, 1:2], op0=ALU.subtract,
                                          op1=ALU.mult), [key, mk], [key])
    S.op("pool", lambda e: e.tensor_tensor(out=zz[:], in0=zz[:], in1=gam, op=ALU.mult), [key, "lnb"], [key])
    S.op("pool", lambda e: e.tensor_tensor(out=dst, in0=zz[:], in1=bet, op=ALU.add), [key, "lnb"], [dkey])


_NC_CACHE = {}


def kernel(**inputs):
    x = np.ascontiguousarray(inputs["x"], dtype=np.float32)
    B, T, _ = x.shape
    NG = T // 256
    key = (NG,)
    if key not in _NC_CACHE:
        _NC_CACHE[key] = build(NG=NG)
    nc = _NC_CACHE[key]
    consts = make_consts()
    c = np.ascontiguousarray(inputs["c"], dtype=np.float32)
    shared = {nm: np.ascontiguousarray(inputs[nm], dtype=np.float32) for nm, _ in WNAMES}
    in_maps = []
    for b in range(B):
        m = {"x": x[b], "c": c[b:b + 1], "consts": consts}
        m.update(shared)
        in_maps.append(m)
    res = run_bass_kernel_spmd(nc, in_maps, core_ids=list(range(B)))
    return np.stack([r["out"] for r in res.results], axis=0).astype(np.float32)
```

```python
import math
import os
import numpy as np
KSKIP = os.environ.get('KSKIP', '')
from contextlib import ExitStack
import concourse.bass as bass
import concourse.mybir as mybir
from concourse.bass_utils import run_bass_kernel_spmd

F32 = mybir.dt.float32
BF16 = mybir.dt.bfloat16
AF = mybir.ActivationFunctionType
ALU = mybir.AluOpType
AX = mybir.AxisListType

COMPUTE = ("pe", "act", "dve", "pool")
ALLQ = ("pe", "act", "dve", "pool", "sp")

D = 1024
IN_DIM = 2444
NE = 32
ALPHA = 4 ** 0.25
BIG = 30000.0


class Sched:
    def __init__(self, nc, stack):
        self.nc = nc
        self.stack = stack
        self.ops = []
        self.res = {}
        self.bar = set()
        self.lastq = {}
        self.dmas_since_bar = []
        self.alias = {}

    def _add(self, q, fn, reads, writes, dma=False):
        reads = tuple(self.alias.get(r, r) for r in reads)
        writes = tuple(self.alias.get(w, w) for w in writes)
        deps = set(self.bar)
        for r in reads:
            st = self.res.get(r)
            if st is not None and st[0] is not None:
                deps.add(st[0])
            if st is not None and isinstance(r, tuple) and r[0] in ("pf", "ph"):
                for rq, ro in st[1].items():
                    if rq != q:
                        deps.add(ro)
        for w in writes:
            st = self.res.get(w)
            if st is not None:
                if st[0] is not None:
                    deps.add(st[0])
                deps.update(st[1].values())
                deps.update(st[2])
        oid = len(self.ops)
        self.ops.append(dict(q=q, fn=fn, deps=deps, dma=dma))
        for r in reads:
            st = self.res.setdefault(r, [None, {}, []])
            if dma:
                st[2].append(oid)
            else:
                st[1][q] = oid
        for w in writes:
            self.res[w] = [oid, {}, []]
        if dma:
            self.dmas_since_bar.append(oid)
        else:
            self.lastq[q] = oid
        return oid

    def op(self, q, fn, reads=(), writes=()):
        return self._add(q, fn, tuple(reads), tuple(writes), dma=False)

    def dma(self, q, out, in_, reads=(), writes=(), slow=False):
        def fn(eng):
            if slow:
                return eng.dma_start(out=out, in_=in_, allow_slow_non_contiguous=True)
            return eng.dma_start(out=out, in_=in_)
        return self._add(q, fn, tuple(reads), tuple(writes), dma=True)

    def barrier(self):
        self.bar = set(self.lastq.values()) | set(self.dmas_since_bar) | set(self.bar)
        self.dmas_since_bar = []

    def emit(self):
        nc = self.nc
        ops = self.ops
        needed = set()
        for o in ops:
            needed.update(o["deps"])
        NLANE = 24
        sems = {q: self.stack.enter_context(nc.semaphore("s_" + q)) for q in COMPUTE}
        lanes = [self.stack.enter_context(nc.semaphore("l_%d" % i)) for i in range(NLANE)]
        lane_val = [0] * NLANE
        cnt = {q: 0 for q in COMPUTE}
        tok = {}
        li = 0
        lane_prev = {}
        for i, o in enumerate(ops):
            if o["dma"]:
                lane = li % NLANE
                li += 1
                lane_val[lane] += 16
                tok[i] = (("L", lane), lane_val[lane])
                if lane in lane_prev:
                    o["deps"] = set(o["deps"]) | {lane_prev[lane]}
                lane_prev[lane] = i
            elif i in needed:
                cnt[o["q"]] += 1
                tok[i] = (("E", o["q"]), cnt[o["q"]])

        def semof(k):
            return lanes[k[1]] if k[0] == "L" else sems[k[1]]

        per_q = {q: [] for q in ALLQ}
        for i, o in enumerate(ops):
            per_q[o["q"]].append(i)
        block = self.stack.enter_context(nc.Block())
        engmap = {}

        def run_q(q, eng):
            seen = {}
            for i in per_q[q]:
                o = ops[i]
                want = {}
                for d in o["deps"]:
                    if d not in tok:
                        continue
                    k, v = tok[d]
                    if (not ops[d]["dma"]) and ops[d]["q"] == q and q == "pe":
                        continue
                    if want.get(k, 0) < v:
                        want[k] = v
                for k, v in want.items():
                    if seen.get(k, 0) < v:
                        eng.wait_ge(semof(k), v)
                        seen[k] = v
                inst = o["fn"](eng)
                if i in tok:
                    k, v = tok[i]
                    inst.then_inc(semof(k), 16 if o["dma"] else 1)
            last = {}
            for i in per_q[q]:
                if ops[i]["dma"]:
                    k, v = tok[i]
                    last[k] = max(last.get(k, 0), v)
            for k, v in last.items():
                if seen.get(k, 0) < v:
                    eng.wait_ge(semof(k), v)

        @block.tensor
        def _(e):
            run_q("pe", e)

        @block.scalar
        def _(e):
            run_q("act", e)

        @block.vector
        def _(e):
            run_q("dve", e)

        @block.gpsimd
        def _(e):
            run_q("pool", e)

        @block.sync
        def _(e):
            run_q("sp", e)


def t5_bucket_np(n):
    max_exact = 16
    nf = np.maximum(n, 1).astype(np.float32)
    large = max_exact + (np.log(nf / max_exact) / math.log(128 / max_exact) * (32 - max_exact)).astype(np.int32)
    large = np.minimum(large, 31)
    return np.where(n < max_exact, n, large)


CW_IDENT = 0
CW_ONES = 128
CW_U = 256
CW_NS = 320
CW_MB = 832
CW_I8 = 1344
CW_OH = 1856
CW_PF = 2240
CW_TOT = 2272


def make_consts():
    c = np.zeros((128, CW_TOT), np.float32)
    c[:, CW_IDENT:CW_IDENT + 128] = np.eye(128, dtype=np.float32)
    c[:, CW_ONES:CW_ONES + 128] = 1.0
    k = np.arange(64)[:, None]
    i = np.arange(64)[None, :]
    c[:64, CW_U:CW_U + 64] = (k <= i)
    ns = np.where(i > k, -1.0, 0.0)
    mb = np.where(i < k, -BIG, 0.0)
    c[:64, CW_NS:CW_NS + 512] = np.tile(ns, (1, 8))
    c[:64, CW_MB:CW_MB + 512] = np.tile(mb, (1, 8))
    c[:64, CW_I8:CW_I8 + 512] = np.tile(np.eye(64), (1, 8))
    dist = np.arange(384) - 127
    valid = (dist >= 0) & (dist < 128)
    bk = t5_bucket_np(np.maximum(dist, 0))
    oh = np.zeros((33, 384), np.float32)
    for ii in range(384):
        if valid[ii]:
            oh[bk[ii], ii] = 1.0
        else:
            oh[32, ii] = -BIG
    c[:33, CW_OH:CW_OH + 384] = oh
    t = np.arange(16)
    for ci, (wa, wb) in enumerate(((2, 4), (8, 16))):
        c[:64, CW_PF + ci * 16:CW_PF + ci * 16 + 16] = wa / np.minimum(t + 1, wa)
        c[64:, CW_PF + ci * 16:CW_PF + ci * 16 + 16] = wb / np.minimum(t + 1, wb)
    return c


WNAMES = [("rel_bias", [32, 6]), ("w_in", [2, D, IN_DIM]), ("w_out", [2, D, D]), ("w_ada", [2, D, 6 * D]),
          ("b_ada", [2, 6 * D]), ("ln1_g", [2, D]), ("ln1_b", [2, D]), ("ln2_g", [2, D]), ("ln2_b", [2, D]),
          ("pool_w", [2, 4, 64, 64]), ("pool_scale", [2, 256]), ("attn_sinks", [2, 6]), ("conv_w", [2, 4, 1152]),
          ("gdn_a_log", [2, 6]), ("gdn_dt_bias", [2, 6]), ("gdn_norm_w", [2, 64]), ("router_w", [2, D, NE]),
          ("router_b", [2, NE]), ("exp_w_up", [2, NE, D, 2 * D]), ("exp_b_up", [2, NE, 2 * D]),
          ("exp_w_down", [2, NE, D, D]), ("exp_b_down", [2, NE, D])]


def build(NG=8, NL=2, do_moe=True, n_exp=NE, ymask=(1, 1, 1)):
    T = NG * 256
    NT = T // 128
    nc = bass.Bass("TRN2", target_bir_lowering=False)
    dr = {}
    dr["x"] = nc.dram_tensor("x", [T, D], F32, kind="ExternalInput").ap()
    dr["c"] = nc.dram_tensor("c", [1, D], F32, kind="ExternalInput").ap()
    dr["consts"] = nc.dram_tensor("consts", [128, CW_TOT], F32, kind="ExternalInput").ap()
    for nm, shp in WNAMES:
        if nm.startswith("exp_w") and not do_moe:
            continue
        dr[nm] = nc.dram_tensor(nm, shp, F32, kind="ExternalInput").ap()
    out = nc.dram_tensor("out", [T, D], F32, kind="ExternalOutput").ap()
    zscr = nc.dram_tensor("zscr", [6, 128, 384], F32, kind="ExternalOutput").ap()
    xs = nc.dram_tensor("xs", [T, D], F32, kind="ExternalOutput").ap()
    dbg = nc.dram_tensor("dbg", [128, 1536], F32, kind="ExternalOutput").ap() if not do_moe else None

    with ExitStack() as st:
        S = Sched(nc, st)
        base = [((nc.sbuf_base + 63) // 64) * 64]
        top = nc.sbuf_top

        acache = {}

        def alloc(name, shape, dt):
            nbytes = int(np.prod(shape[1:])) * (2 if dt == BF16 else 4)
            nbytes = (nbytes + 63) // 64 * 64
            off = base[0]
            base[0] += nbytes
            assert base[0] <= top, ("SBUF overflow", name, base[0], top)
            ck = (name, tuple(shape), str(dt), off)
            if ck not in acache:
                acache[ck] = nc.alloc_sbuf_tensor_at(name, list(shape), dt, offset=off)
            return acache[ck]

        pfall = st.enter_context(nc.psum_tensor("pfall", [128, 4096], F32))
        pf = [pfall[:, i * 512:(i + 1) * 512] for i in range(8)]
        rr = [0, 0]

        def pbank():
            i = rr[0] % 7
            rr[0] += 1
            return pf[i], ("pf", i)

        def pbank2():
            i = (rr[1] % 3) * 2
            rr[1] += 1
            return pfall[:, i * 512:(i + 2) * 512], [("pf", i), ("pf", i + 1)]

        def mm(out_, lhsT, rhs, start, stop, reads, writes):
            S.op("pe", lambda e: e.matmul(out_, lhsT=lhsT, rhs=rhs, start=start, stop=stop), reads, writes)

        def tr(out_, in_, ident_, reads, writes):
            S.op("pe", lambda e: e.transpose(out=out_, in_=in_, identity=ident_), reads, writes)

        def act(out_, in_, func, reads, writes, scale=None, bias=None):
            kw = {}
            if scale is not None:
                kw["scale"] = scale
            if bias is not None:
                kw["bias"] = bias
            S.op("act", lambda e: e.activation(out=out_, in_=in_, func=func, **kw), reads, writes)

        def tt(q, out_, a, b, op, reads, writes):
            S.op(q, lambda e: e.tensor_tensor(out=out_, in0=a, in1=b, op=op), reads, writes)

        def ts(q, out_, a, s1, s2, op0, op1, reads, writes):
            if op1 is None:
                S.op(q, lambda e: e.tensor_scalar(out=out_, in0=a, scalar1=s1, scalar2=None, op0=op0), reads, writes)
            else:
                S.op(q, lambda e: e.tensor_scalar(out=out_, in0=a, scalar1=s1, scalar2=s2, op0=op0, op1=op1), reads, writes)

        def stt(q, out_, a, sc, b, op0, op1, reads, writes):
            S.op(q, lambda e: e.scalar_tensor_tensor(out=out_, in0=a, scalar=sc, in1=b, op0=op0, op1=op1), reads, writes)

        def cp(q, out_, in_, reads, writes):
            S.op(q, lambda e: e.tensor_copy(out=out_, in_=in_), reads, writes)

        def memset(q, ap, val, writes):
            S.op(q, lambda e: e.memset(ap, val), (), writes)

        stg = {}
        stg_i = [0]

        def load_cast(dst, src, w, key, mul=None, srcs=None):
            i = stg_i[0] % len(stg["bufs"])
            stg_i[0] += 1
            sbuf = stg["bufs"][i]
            sk = "stg%d" % i
            if srcs is None:
                S.dma("sp", sbuf[:, :, 0:w], src.rearrange("(k p) n -> p k n", p=128), writes=[(sk, 0), (sk, 128)])
                sks = [(sk, 0), (sk, 128)]
            else:
                sks = []
                for (c0, ncol, ap) in srcs:
                    S.dma("sp", sbuf[:, :, c0:c0 + ncol], ap.rearrange("(k p) n -> p k n", p=128), writes=[(sk, c0)])
                    sks.append((sk, c0))
            if mul is None:
                if stg_i[0] % 2 == 0:
                    act(dst, sbuf[:, :, 0:w], AF.Copy, sks, [key])
                else:
                    cp("pool", dst, sbuf[:, :, 0:w], sks, [key])
                return [key]
            mb = mul.unsqueeze(1).to_broadcast([128, 4, w])
            tt("pool", dst[:, 0:4, :], sbuf[:, 0:4, 0:w], mb, ALU.mult, sks + ["gbc"], [(key, 0)])
            tt("dve", dst[:, 4:8, :], sbuf[:, 4:8, 0:w], mb, ALU.mult, sks + ["gbc"], [(key, 1)])
            return [(key, 0), (key, 1)]

        cst = alloc("cst", [128, CW_TOT], F32)
        ident = cst[:, CW_IDENT:CW_IDENT + 128]
        ones = cst[:, CW_ONES:CW_ONES + 128]
        U64 = cst[0:64, CW_U:CW_U + 64]
        NS8 = cst[0:64, CW_NS:CW_NS + 512]
        MB8 = cst[0:64, CW_MB:CW_MB + 512]
        I8 = cst[0:64, CW_I8:CW_I8 + 512]
        OH = cst[0:33, CW_OH:CW_OH + 384]
        identb = alloc("identb", [128, 128], BF16)
        ca = alloc("ca", [128, 8], F32)
        modcol = alloc("modcol", [128, 4, 8], F32)
        gbc = alloc("gbc", [128, 2, D], F32)
        rowt = alloc("rowt", [1, 1024], F32)
        rowm = alloc("rowm", [1, 512], F32)
        small = alloc("small", [128, 128], F32)
        phase_base = base[0]

        S.dma("sp", cst[:], dr["consts"][:, :], writes=["cst"])
        cp("dve", identb[:], ident, ["cst"], ["identb"])
        S.dma("sp", ca[:], dr["c"][0, :].rearrange("(k p) -> p k", p=128), writes=["ca"], slow=True)
        act(ca[:], ca[:], AF.Silu, ["ca"], ["ca"])

        def bcast_row(dst, src_row, n, key):
            S.dma("sp", rowt[0:1, 0:n], src_row, writes=["rowt"])
            for h0 in range(0, n, 512):
                w = min(512, n - h0)
                pb, pk = pbank()
                mm(pb[:, 0:w], ones[0:1, 0:128], rowt[0:1, h0:h0 + w], True, True, ["cst", "rowt"], [pk])
                act(dst[:, h0:h0 + w], pb[:, 0:w], AF.Copy, [pk], [key])

        rbT = alloc("rbT", [33, 6, 128], F32)
        rb = alloc("rb", [32, 6], F32)
        Fb = alloc("Fb", [128, 6, 384], F32)
        S.dma("sp", rb[:], dr["rel_bias"][:, :], writes=["rb"])
        memset("dve", rbT[:], 1.0, ["rbT"])
        cp("dve", rbT[0:32, :, :], rb[:].unsqueeze(2).to_broadcast([32, 6, 128]), ["rb", "rbT"], ["rbT"])
        for h in range(6):
            pb, pk = pbank()
            mm(pb[:, 0:384], rbT[:, h, :], OH, True, True, ["rbT", "cst"], [pk])
            act(Fb[:, h, :], pb[:, 0:384], AF.Copy, [pk], ["Fb"])
        S.dma("sp", zscr.rearrange("h p i -> p h i"), Fb[:], reads=["Fb"], writes=["zscr"])
        base[0] = phase_base
        S.barrier()

        for l in range(NL):
            base[0] = phase_base
            was = [alloc("wa0", [128, 8, 512], F32), alloc("wa1", [128, 8, 512], F32)]
            for n in range(12):
                wa = was[n % 2]
                wak = "wa%d" % (n % 2)
                S.dma("sp", wa[:], dr["w_ada"][l, :, n * 512:(n + 1) * 512].rearrange("(k p) n -> p k n", p=128),
                      writes=[wak])
                S.dma("sp", rowt[0:1, 0:512], dr["b_ada"][l:l + 1, n * 512:(n + 1) * 512], writes=["rowt"])
                pb, pk = pbank()
                for kc in range(8):
                    mm(pb[0:1, :], ca[:, kc:kc + 1], wa[:, kc, :], kc == 0, False, ["ca", wak], [pk])
                mm(pb[0:1, :], ones[0:1, 0:1], rowt[0:1, 0:512], False, True, ["cst", "rowt"], [pk])
                act(rowm[0:1, :], pb[0:1, :], AF.Copy, [pk], ["rowm"])
                which = n // 2
                half = n % 2
                if which in (0, 1, 3, 4):
                    slot = {0: 0, 1: 1, 3: 2, 4: 3}[which]
                    pb2, pk2 = pbank()
                    for j in range(4):
                        mm(pb2[:, j:j + 1], rowm[0:1, j * 128:(j + 1) * 128], ones[0:1, 0:1], True, True,
                           ["rowm", "cst"], [pk2])
                    if slot in (1, 3):
                        ts("dve", modcol[:, slot, half * 4:half * 4 + 4], pb2[:, 0:4], 1.0, None, ALU.add, None,
                           [pk2], ["modcol"])
                    else:
                        cp("dve", modcol[:, slot, half * 4:half * 4 + 4], pb2[:, 0:4], [pk2], ["modcol"])
                else:
                    gi = 0 if which == 2 else 1
                    pb2, pk2 = pbank()
                    mm(pb2[:, :], ones[0:1, 0:128], rowm[0:1, :], True, True, ["cst", "rowm"], [pk2])
                    act(gbc[:, gi, half * 512:(half + 1) * 512], pb2[:, :], AF.Copy, [pk2], ["gbc"])
            S.barrier()

            base[0] = phase_base
            lnb = alloc("lnb", [128, 2, D], F32)
            for i, nm in enumerate(("ln1_g", "ln1_b")):
                bcast_row(lnb[:, i, :], dr[nm][l:l + 1, :], 1024, "lnb")
            BM = alloc("BM", [128, 6, 256], F32)
            S.dma("sp", BM[:], bass.AP(tensor=zscr.tensor, offset=127, ap=[[383, 128], [128 * 384, 6], [1, 256]]),
                  reads=["zscr"], writes=["BM"])
            win = alloc("win", [128, 8, IN_DIM], BF16)
            wout = alloc("wout", [128, 8, D], BF16)
            stg_mark = base[0]
            stg["bufs"] = [alloc("stgA", [128, 8, 256], F32), alloc("stgB", [128, 8, 256], F32)]
            stg_end = base[0]
            for c0 in range(0, IN_DIM, 256):
                w = min(256, IN_DIM - c0)
                load_cast(win[:, :, c0:c0 + w], dr["w_in"][l, :, c0:c0 + w], w, "win")
            woutk = []
            for c0 in range(0, D, 256):
                woutk += load_cast(wout[:, :, c0:c0 + 256], dr["w_out"][l, :, c0:c0 + 256], 256, ("wout", c0), mul=gbc[:, 0, c0:c0 + 256])
            base[0] = stg_mark
            raw = alloc("raw", [128, 9, 260], F32)
            fm = alloc("fm", [128, 9, 256], F32)
            assert base[0] >= stg_end
            poolW = alloc("poolW", [128, 2, 128], F32)
            poolWb = alloc("poolWb", [128, 2, 128], BF16)
            presb = alloc("presb", [128, 256], BF16)
            memset("dve", poolW[:], 0.0, ["poolW"])
            for gi in range(4):
                ci, hf = gi // 2, gi % 2
                S.dma("sp", poolW[hf * 64:(hf + 1) * 64, ci, hf * 64:(hf + 1) * 64], dr["pool_w"][l, gi, :, :],
                      reads=["poolW"], writes=["poolW%d" % gi])
            cp("dve", poolWb[:], poolW[:], ["poolW", "poolW0", "poolW1", "poolW2", "poolW3"], ["poolWb"])
            pscale = alloc("pscale", [128, 2], F32)
            S.dma("sp", pscale[:], dr["pool_scale"][l, :].rearrange("(c p) -> p c", p=128), writes=["pscale"], slow=True)
            convw = alloc("convw", [128, 9, 4], F32)
            for j in range(4):
                S.dma("sp", convw[:, :, j], dr["conv_w"][l, j, :].rearrange("(c p) -> p c", p=128), writes=["convw%d" % j], slow=True)
            esink = alloc("esink", [128, 6], F32)
            bcast_row(esink[:, :], dr["attn_sinks"][l:l + 1, :], 6, "esink")
            act(esink[:], esink[:], AF.Exp, ["esink"], ["esink"])
            gpar = alloc("gpar", [64, 3, 64], F32)
            bcast_row(small[:, 0:6], dr["gdn_a_log"][l:l + 1, :], 6, "small")
            act(gpar[:, 0, 0:6], small[0:64, 0:6], AF.Exp, ["small"], ["gpar0"])
            ts("dve", gpar[:, 0, 0:6], gpar[:, 0, 0:6], -1.0, None, ALU.mult, None, ["gpar0"], ["gpar0"])
            bcast_row(small[:, 8:14], dr["gdn_dt_bias"][l:l + 1, :], 6, "small2")
            cp("dve", gpar[:, 1, 0:6], small[0:64, 8:14], ["small2"], ["gpar1"])
            bcast_row(small[:, 64:128], dr["gdn_norm_w"][l:l + 1, :], 64, "small3")
            cp("dve", gpar[:, 2, :], small[0:64, 64:128], ["small3"], ["gpar2"])

            xg = alloc("xg", [128, 2, D], F32)
            hT = alloc("hT", [128, 8, 256], BF16)
            yT = alloc("yT", [128, 8, 256], BF16)
            ubuf = alloc("ubuf", [128, 2, 272], F32)
            qTa = alloc("qTa", [64, 6, 256], BF16)
            kTa = alloc("kTa", [64, 2, 384], BF16)
            Vaug = alloc("Vaug", [128, 3, 2, 65], BF16)
            sT = alloc("sT", [128, 256], F32)
            eT = alloc("eT", [128, 256], BF16)
            onrm = alloc("onrm", [128, 384], F32)
            rden = alloc("rden", [128, 6], F32)
            cacc = alloc("cacc", [128, 256], F32)
            ztm = alloc("ztm", [64, 4, 384], F32)
            batm = alloc("batm", [64, 4, 12], F32)
            zz_off = base[0]
            zzb = alloc("zz", [128, 1088], F32)
            zz = zzb[:, 0:D]
            if ("ptmp", zz_off) not in acache:
                acache[("ptmp", zz_off)] = nc.alloc_sbuf_tensor_at("ptmp", [128, 4, 272], F32, offset=zz_off)
            ptmp = acache[("ptmp", zz_off)]
            S.alias["ptmp"] = "zz"
            stats = alloc("stats", [128, 2, 6], F32)
            mv = alloc("mv", [128, 2], F32)
            G = {nm: alloc("g_" + nm, [64, 12, 64], F32) for nm in
                 ("q", "k", "v", "kb", "kbg", "qd", "kd", "kT", "kbT", "qT", "qdT", "DT", "A", "N", "P", "attT", "sq")}
            for al, cn in (("A2", "q"), ("N2", "k"), ("u", "kb"), ("wT", "kT"), ("vn", "kbT"), ("o", "qd")):
                G[al] = G[cn]
                S.alias["g_" + al] = "g_" + cn
                S.alias[("g_" + al, 0)] = "g_" + cn
                S.alias[("g_" + al, 1)] = "g_" + cn
            Sst = alloc("Sst", [64, 6, 64], F32)
            gsm = alloc("gsm", [64, 12, 12], F32)
            S.barrier()
            memset("dve", Sst[:], 0.0, ["Sst"])
            memset("dve", ubuf[:], 0.0, ["ubuf"])
            memset("dve", raw[:], 0.0, ["raw"])
            memset("dve", kTa[:], 0.0, ["kTa"])
            memset("dve", Vaug[:], 0.0, ["Vaug"])
            memset("dve", Vaug[:, :, :, 64:65], 1.0, ["Vaug"])

            C_POOL, C_Q, C_K, C_V, C_G, C_Z, C_B = 0, 256, 640, 768, 896, 2048, 2432

            xsrc = dr["x"] if l == 0 else xs
            for g in range(NG):
                t0 = 2 * g
                for i in range(2):
                    S.dma("sp", xg[:, i, :], xsrc[(t0 + i) * 128:(t0 + i + 1) * 128, :], reads=[("xs", t0 + i)], writes=[("xg", i)])
                for kc in range(8):
                    if kc % 2 == 0:
                        pb, pk = pbank()
                    for i in range(2):
                        tr(pb[:, (kc % 2) * 256 + i * 128:(kc % 2) * 256 + (i + 1) * 128],
                           xg[:, i, kc * 128:(kc + 1) * 128], ident, [("xg", i), "cst"], [pk])
                    act(hT[:, kc, :], pb[:, (kc % 2) * 256:(kc % 2 + 1) * 256], AF.Identity, [pk, "modcol"], ["hT"],
                        scale=modcol[:, 1, kc:kc + 1], bias=modcol[:, 0, kc:kc + 1])

                def proj_fm(col0, M, evac):
                    pb, pk = pbank()
                    for kc in range(8):
                        mm(pb[0:M, 0:256], win[:, kc, col0:col0 + M], hT[:, kc, :], kc == 0, kc == 7, ["win", "hT"], [pk])
                    evac(pb, pk)

                for ci in range(2):
                    proj_fm(C_POOL + ci * 128, 128,
                            lambda pb, pk, ci=ci: act(ubuf[:, ci, 16:272], pb[:, 0:256], AF.Copy, [pk], ["ubuf"]))
                for h in range(6):
                    proj_fm(C_Q + h * 64, 64,
                            lambda pb, pk, h=h: act(qTa[:, h, :], pb[0:64, 0:256], AF.Copy, [pk], ["qTa"]))
                for h in range(2):
                    proj_fm(C_K + h * 64, 64,
                            lambda pb, pk, h=h: act(kTa[:, h, 128:384], pb[0:64, 0:256], AF.Copy, [pk], ["kTa"]))
                for ci in range(9):
                    proj_fm(C_G + ci * 128, 128,
                            lambda pb, pk, ci=ci: cp("dve", raw[:, ci, 4:260], pb[:, 0:256], [pk], ["raw"]))
                for i in range(2):
                    pb, pk = pbank()
                    for kc in range(8):
                        mm(pb[:, 0:128], hT[:, kc, i * 128:(i + 1) * 128], win[:, kc, C_V:C_V + 128], kc == 0, kc == 7,
                           ["win", "hT"], [pk])
                    act(Vaug[:, 1 + i, :, 0:64], pb[:, 0:128].rearrange("p (g d) -> p g d", g=2), AF.Copy, [pk], ["Vaug"])
                for cch in range(4):
                    pb, pk = pbank()
                    for kc in range(8):
                        mm(pb[0:64, 0:396], hT[:, kc, cch * 64:(cch + 1) * 64], win[:, kc, C_Z:C_Z + 396], kc == 0, kc == 7,
                           ["win", "hT"], [pk])
                    act(ztm[:, cch, :], pb[0:64, 0:384], AF.Silu, [pk], ["ztm"])
                    cp("dve", batm[:, cch, :], pb[0:64, 384:396], [pk], ["batm"])

                for ci in (range(2) if 'p' not in KSKIP else ()):
                    u_ = ubuf[:, ci, :]
                    s2, s4, s8, s16 = (ptmp[:, j, :] for j in range(4))
                    tt("dve", s2[:, 1:272], u_[:, 1:272], u_[:, 0:271], ALU.add, ["ubuf"], ["ptmp"])
                    tt("dve", s4[:, 3:272], s2[:, 3:272], s2[:, 1:270], ALU.add, ["ptmp"], ["ptmp"])
                    if ci == 1:
                        tt("dve", s8[:, 7:272], s4[:, 7:272], s4[:, 3:268], ALU.add, ["ptmp"], ["ptmp"])
                        tt("dve", s16[:, 15:272], s8[:, 15:272], s8[:, 7:264], ALU.add, ["ptmp"], ["ptmp"])
                        lo, hi, wl, wh = s8, s16, 8.0, 16.0
                    else:
                        lo, hi, wl, wh = s2, s4, 2.0, 4.0
                    pres = presb
                    if g == 0:
                        tt("dve", lo[0:64, 16:32], lo[0:64, 16:32], cst[0:64, CW_PF + ci * 16:CW_PF + ci * 16 + 16],
                           ALU.mult, ["ptmp", "cst"], ["ptmp"])
                        tt("dve", hi[64:128, 16:32], hi[64:128, 16:32],
                           cst[64:128, CW_PF + ci * 16:CW_PF + ci * 16 + 16], ALU.mult, ["ptmp", "cst"], ["ptmp"])
                    stt("dve", pres[0:64, :], lo[0:64, 16:272], 1.0 / wl, u_[0:64, 16:272], ALU.mult, ALU.subtract,
                        ["ptmp", "ubuf"], ["presb"])
                    stt("dve", pres[64:128, :], hi[64:128, 16:272], 1.0 / wh, u_[64:128, 16:272], ALU.mult, ALU.subtract,
                        ["ptmp", "ubuf"], ["presb"])
                    if 'x' not in KSKIP:
                        pb, pk = pbank()
                        mm(pb[:, 0:256], poolWb[:, ci, :], pres[:, :], True, True, ["poolWb", "presb"], [pk])
                        if 'w' not in KSKIP:
                            act(yT[:, ci, :], pb[:, 0:256], AF.Identity, [pk, "pscale"], ["yT"], scale=pscale[:, ci:ci + 1])
                        else:
                            act(yT[:, ci, :], pb[:, 0:256], AF.Copy, [pk, "pscale"], ["yT"])
                    cp("dve", ubuf[:, ci, 0:16], ubuf[:, ci, 256:272], ["ubuf", "presb"], ["ubuf"])

                for i in (range(2) if 'a' not in KSKIP else ()):
                    pbo, pko = pf[7], ("pf", 7)
                    for h in range(6):
                        gk = h // 3
                        pb, pk = pbank()
                        first = (g == 0 and i == 0)
                        mm(pb[:, 0:128], kTa[:, gk, 128 + i * 128:256 + i * 128], qTa[:, h, i * 128:(i + 1) * 128],
                           True, True, ["kTa", "qTa"], [pk])
                        if not first:
                            mm(pb[:, 128:256], kTa[:, gk, i * 128:128 + i * 128], qTa[:, h, i * 128:(i + 1) * 128],
                               True, True, ["kTa", "qTa"], [pk])
                        ncol = 128 if first else 256
                        stt("dve", sT[:, 0:ncol], pb[:, 0:ncol], 0.125, BM[:, h, 0:ncol], ALU.mult, ALU.add,
                            [pk, "BM"], ["sT"])
                        act(eT[:, 0:ncol], sT[:, 0:ncol], AF.Exp, ["sT"], ["eT"])
                        mm(pbo[:, h * 65:(h + 1) * 65], eT[:, 0:128], Vaug[:, 1 + i, gk, :], True, first,
                           ["eT", "Vaug"], [pko])
                        if not first:
                            mm(pbo[:, h * 65:(h + 1) * 65], eT[:, 128:256], Vaug[:, i, gk, :], False, True,
                               ["eT", "Vaug"], [pko])
                    po3 = pbo[:, 0:390].rearrange("p (h e) -> p h e", h=6)
                    tt("dve", rden[:], po3[:, :, 64], esink[:], ALU.add, [pko, "esink"], ["rden"])
                    S.op("dve", lambda e: e.reciprocal(out=rden[:], in_=rden[:]), ["rden"], ["rden"])
                    tt("dve", onrm[:].rearrange("p (h d) -> p h d", h=6), po3[:, :, 0:64],
                       rden[:].unsqueeze(2).to_broadcast([128, 6, 64]), ALU.mult, [pko, "rden"], ["onrm"])
                    if dbg is not None and g == 0 and i == 0:
                        S.dma("sp", dbg[:, 0:384], onrm[:], reads=["onrm"])
                        S.dma("sp", dbg[:, 384:390], rden[:], reads=["rden"])
                        S.dma("sp", dbg[:, 400:528], sT[:, 0:128], reads=["sT"])
                    hb, hk = pbank()
                    for c3 in range(3):
                        tr(hb[:, c3 * 128:(c3 + 1) * 128], onrm[:, c3 * 128:(c3 + 1) * 128], ident, ["onrm", "cst"], [hk])
                    act(yT[:, 2:5, i * 128:(i + 1) * 128], hb[:, 0:384].rearrange("p (c t) -> p c t", c=3), AF.Copy,
                        [hk], ["yT"])
                cp("dve", kTa[:, :, 0:128], kTa[:, :, 256:384], ["kTa"], ["kTa"])
                cp("dve", Vaug[:, 0, :, :], Vaug[:, 2, :, :], ["Vaug"], ["Vaug"])

                for ci in (range(9) if 'c' not in KSKIP else ()):
                    ts("dve", cacc[:], raw[:, ci, 4:260], convw[:, ci, 3:4], None, ALU.mult, None, ["raw", "convw0", "convw1", "convw2", "convw3"], ["cacc"])
                    for j in range(3):
                        stt("dve", cacc[:], raw[:, ci, 1 + j:257 + j], convw[:, ci, j:j + 1], cacc[:], ALU.mult, ALU.add,
                            ["raw", "convw0", "convw1", "convw2", "convw3", "cacc"], ["cacc"])
                    act(fm[:, ci, :], cacc[:], AF.Silu, ["cacc"], ["fm"])
                cp("dve", raw[:, :, 0:4], raw[:, :, 256:260], ["raw", "fm"], ["raw"])

                for cp2 in (range(2) if 'g' not in KSKIP else ()):
                    W = 768

                    def fl(nm):
                        return G[nm][:].rearrange("p m d -> p (m d)")
                    for qi, nm in enumerate(("q", "k", "v")):
                        pb, pk = pbank2()
                        for c2 in range(2):
                            tok0 = (cp2 * 2 + c2) * 64
                            for c3 in range(3):
                                tr(pb[0:64, c2 * 384 + c3 * 128:c2 * 384 + (c3 + 1) * 128], fm[:, qi * 3 + c3, tok0:tok0 + 64],
                                   ident, ["fm", "cst"], pk)
                        act(fl(nm), pb[0:64, 0:W], AF.Copy, pk, ["g_" + nm])
                    for nm, col, scl in (("q", 0, 0.125), ("k", 1, 1.0)):
                        gk_ = "gsm%d" % col
                        tt("dve", G["sq"][:], G[nm][:], G[nm][:], ALU.mult, ["g_" + nm], ["g_sq"])
                        S.op("dve", lambda e, col=col: e.tensor_reduce(out=gsm[:, col, :], in_=G["sq"][:], axis=AX.X,
                                                                       op=ALU.add), ["g_sq"], [gk_])
                        act(gsm[:, col, :], gsm[:, col, :], AF.Sqrt, [gk_], [gk_], bias=1e-6)
                        S.op("dve", lambda e, col=col: e.reciprocal(out=gsm[:, col, :], in_=gsm[:, col, :]), [gk_], [gk_])
                        if scl != 1.0:
                            ts("dve", gsm[:, col, :], gsm[:, col, :], scl, None, ALU.mult, None, [gk_], [gk_])
                        tt("dve", G[nm][:], G[nm][:], gsm[:, col, :].unsqueeze(2).to_broadcast([64, 12, 64]), ALU.mult,
                           ["g_" + nm, gk_], ["g_" + nm])
                    b3 = batm[:, cp2 * 2:cp2 * 2 + 2, :]
                    g2v = lambda col: gsm[:, col, :].rearrange("p (c h) -> p c h", c=2)
                    act(g2v(2), b3[:, :, 0:6], AF.Sigmoid, ["batm"], ["gsm2"])
                    tt("dve", g2v(3), b3[:, :, 6:12], gpar[:, 1, 0:6].unsqueeze(1).to_broadcast([64, 2, 6]), ALU.add,
                       ["batm", "gpar1"], ["gsm3"])
                    act(gsm[:, 3, :], gsm[:, 3, :], AF.Exp, ["gsm3"], ["gsm3"])
                    act(gsm[:, 3, :], gsm[:, 3, :], AF.Ln, ["gsm3"], ["gsm3"], bias=1.0)
                    tt("dve", g2v(3), g2v(3), gpar[:, 0, 0:6].unsqueeze(1).to_broadcast([64, 2, 6]), ALU.mult,
                       ["gsm3", "gpar0"], ["gsm3"])
                    pb, pk = pbank()
                    mm(pb[0:64, 0:12], U64, gsm[:, 3, :], True, True, ["cst", "gsm3"], [pk])
                    mm(pb[0:64, 16:28], ones[0:64, 0:64], gsm[:, 3, :], True, True, ["cst", "gsm3"], [pk])
                    cp("dve", gsm[:, 4, :], pb[0:64, 0:12], [pk], ["gsm4"])
                    cp("dve", gsm[:, 5, :], pb[0:64, 16:28], [pk], ["gsm5"])
                    act(gsm[:, 6, :], gsm[:, 4, :], AF.Exp, ["gsm4"], ["gsm6"])
                    tt("dve", gsm[:, 7, :], gsm[:, 5, :], gsm[:, 4, :], ALU.subtract, ["gsm4", "gsm5"], ["gsm7"])
                    act(gsm[:, 7, :], gsm[:, 7, :], AF.Exp, ["gsm7"], ["gsm7"])
                    act(gsm[:, 8, :], gsm[:, 5, :], AF.Exp, ["gsm5"], ["gsm8"])

                    def bc(col):
                        return gsm[:, col, :].unsqueeze(2).to_broadcast([64, 12, 64])
                    tt("dve", G["kb"][:], G["k"][:], bc(2), ALU.mult, ["g_k", "gsm2"], ["g_kb"])
                    tt("pool", G["v"][:], G["v"][:], bc(2), ALU.mult, ["g_v", "gsm2"], ["g_v"])
                    tt("dve", G["kbg"][:], G["kb"][:], bc(6), ALU.mult, ["g_kb", "gsm6"], ["g_kbg"])
                    tt("pool", G["qd"][:], G["q"][:], bc(6), ALU.mult, ["g_q", "gsm6"], ["g_qd"])
                    tt("pool", G["kd"][:], G["k"][:], bc(7), ALU.mult, ["g_k", "gsm7"], ["g_kd"])
                    for src, dstn in (("k", "kT"), ("kb", "kbT"), ("q", "qT"), ("qd", "qdT")):
                        pb, pk = pbank2()
                        for m in range(12):
                            tr(pb[0:64, m * 64:(m + 1) * 64], G[src][:, m, :], ident[0:64, 0:64], ["g_" + src, "cst"], pk)
                        act(fl(dstn), pb[0:64, 0:W], AF.Copy, pk, ["g_" + dstn])
                    I12 = cst[0:64, CW_I8:CW_I8 + 64].unsqueeze(1).to_broadcast([64, 12, 64])
                    tt("dve", G["sq"][:], I12, bc(4), ALU.mult, ["cst", "gsm4"], ["g_sq"])
                    ts("pool", G["DT"][:], bc(4), -1.0, None, ALU.mult, None, ["gsm4"], ["g_DT"])
                    pb, pk = pbank2()
                    for hb_ in range(3):
                        cs = slice(hb_ * 256, (hb_ + 1) * 256)
                        mm(pb[0:64, cs], ones[0:64, 0:64], fl("sq")[:, cs], True, False, ["cst", "g_sq"], pk)
                        mm(pb[0:64, cs], ident[0:64, 0:64], fl("DT")[:, cs], False, False, ["cst", "g_DT"], pk)
                        mm(pb[0:64, cs], ident[0:64, 0:64], MB8[:, 0:256], False, True, ["cst"], pk)
                    act(fl("DT"), pb[0:64, 0:W], AF.Exp, pk, ["g_DT"])
                    pb, pk = pbank2()
                    pb2, pk2 = pbank2()
                    for m in range(12):
                        mm(pb[0:64, m * 64:(m + 1) * 64], G["kT"][:, m, :], G["kbT"][:, m, :], True, True,
                           ["g_kT", "g_kbT"], pk)
                        mm(pb2[0:64, m * 64:(m + 1) * 64], G["kT"][:, m, :], G["qT"][:, m, :], True, True,
                           ["g_kT", "g_qT"], pk2)
                    NS12 = cst[0:64, CW_NS:CW_NS + 64].unsqueeze(1).to_broadcast([64, 12, 64])
                    tt("dve", fl("A"), pb[0:64, 0:W], fl("DT"), ALU.mult, pk + ["g_DT"], ["g_A"])
                    tt("pool", G["A"][:], G["A"][:], NS12, ALU.mult, ["g_A", "cst"], ["g_A"])
                    tt("dve", fl("attT"), pb2[0:64, 0:W], fl("DT"), ALU.mult, pk2 + ["g_DT"], ["g_attT"])
                    pb, pk = pbank2()
                    for m in range(12):
                        tr(pb[0:64, m * 64:(m + 1) * 64], G["A"][:, m, :], ident[0:64, 0:64], ["g_A", "cst"], pk)
                    act(fl("N"), pb[0:64, 0:W], AF.Copy, pk, ["g_N"])
                    tt("pool", G["P"][:], G["A"][:], I12, ALU.add, ["g_A", "cst"], ["g_P"])
                    An, Nn, Ao, No = "A", "N", "A2", "N2"
                    for lev in range(1, 6):
                        pbn, pkn = pbank2()
                        for m in range(12):
                            mm(pbn[0:64, m * 64:(m + 1) * 64], G[An][:, m, :], G[Nn][:, m, :], True, True,
                               ["g_" + An, "g_" + Nn], pkn)
                        act(fl(No), pbn[0:64, 0:W], AF.Copy, pkn, ["g_" + No])
                        if lev < 5:
                            pba, pka = pbank2()
                            for m in range(12):
                                mm(pba[0:64, m * 64:(m + 1) * 64], G[Nn][:, m, :], G[An][:, m, :], True, True,
                                   ["g_" + An, "g_" + Nn], pka)
                            cp("dve", fl(Ao), pba[0:64, 0:W], pka, ["g_" + Ao])
                        pbp, pkp = pbank2()
                        for m in range(12):
                            mm(pbp[0:64, m * 64:(m + 1) * 64], G[No][:, m, :], G["P"][:, m, :], True, True,
                               ["g_" + No, "g_P"], pkp)
                        tt("dve", fl("P"), fl("P"), pbp[0:64, 0:W], ALU.add, ["g_P"] + pkp, ["g_P"])
                        An, Ao = Ao, An
                        Nn, No = No, Nn
                    pb, pk = pbank2()
                    pb2, pk2 = pbank2()
                    for m in range(12):
                        mm(pb[0:64, m * 64:(m + 1) * 64], G["P"][:, m, :], G["v"][:, m, :], True, True, ["g_P", "g_v"], pk)
                        mm(pb2[0:64, m * 64:(m + 1) * 64], G["kbg"][:, m, :], G["P"][:, m, :], True, True,
                           ["g_P", "g_kbg"], pk2)
                    act(fl("u"), pb[0:64, 0:W], AF.Copy, pk, ["g_u"])
                    cp("dve", fl("wT"), pb2[0:64, 0:W], pk2, ["g_wT"])
                    for c2 in range(2):
                        cch = cp2 * 2 + c2
                        tok0 = cch * 64
                        ms = slice(c2 * 6, c2 * 6 + 6)
                        cs = slice(c2 * 384, (c2 + 1) * 384)
                        pb, pk = pbank()
                        for h in range(6):
                            mm(pb[0:64, h * 64:(h + 1) * 64], G["wT"][:, c2 * 6 + h, :], Sst[:, h, :], True, True,
                               ["g_wT", "Sst"], [pk])
                        tt("dve", fl("vn")[:, cs], fl("u")[:, cs], pb[0:64, 0:384], ALU.subtract, ["g_u", pk], [("g_vn", c2)])
                        pbo, pko = pbank()
                        pbs, pks = pbank()
                        for h in range(6):
                            m = c2 * 6 + h
                            mm(pbo[0:64, h * 64:(h + 1) * 64], G["qdT"][:, m, :], Sst[:, h, :], True, False, ["g_qdT", "Sst"], [pko])
                            mm(pbo[0:64, h * 64:(h + 1) * 64], G["attT"][:, m, :], G["vn"][:, m, :], False, True,
                               ["g_attT", ("g_vn", c2)], [pko])
                            mm(pbs[0:64, h * 64:(h + 1) * 64], G["kd"][:, m, :], G["vn"][:, m, :], True, True,
                               ["g_kd", ("g_vn", c2)], [pks])
                        tt("dve", Sst[:], Sst[:], gsm[:, 8, c2 * 6:c2 * 6 + 6].unsqueeze(2).to_broadcast([64, 6, 64]), ALU.mult,
                           ["Sst", "gsm8"], ["Sst"])
                        tt("dve", Sst[:].rearrange("p h d -> p (h d)"), Sst[:].rearrange("p h d -> p (h d)"), pbs[0:64, 0:384],
                           ALU.add, ["Sst", pks], ["Sst"])
                        act(fl("o")[:, cs], pbo[0:64, 0:384], AF.Copy, [pko], [("g_o", c2)])
                    tt("pool", G["sq"][:], G["o"][:], G["o"][:], ALU.mult, [("g_o", 0), ("g_o", 1)], ["g_sq"])
                    S.op("dve", lambda e: e.tensor_reduce(out=gsm[:, 9, :], in_=G["sq"][:], axis=AX.X, op=ALU.add),
                         ["g_sq"], ["gsm9"])
                    act(gsm[:, 9, :], gsm[:, 9, :], AF.Sqrt, ["gsm9"], ["gsm9"], scale=1.0 / 64, bias=1e-6)
                    S.op("dve", lambda e: e.reciprocal(out=gsm[:, 9, :], in_=gsm[:, 9, :]), ["gsm9"], ["gsm9"])
                    tt("dve", G["o"][:], G["o"][:], bc(9), ALU.mult, [("g_o", 0), ("g_o", 1), "gsm9"], [("g_o", 0), ("g_o", 1)])
                    tt("pool", G["o"][:], G["o"][:], gpar[:, 2, :].unsqueeze(1).to_broadcast([64, 12, 64]), ALU.mult,
                       [("g_o", 0), ("g_o", 1), "gpar2"], [("g_o", 0), ("g_o", 1)])
                    tt("dve", fl("sq"), fl("o"), ztm[:, cp2 * 2:cp2 * 2 + 2, :].rearrange("p c f -> p (c f)"), ALU.mult,
                       [("g_o", 0), ("g_o", 1), "ztm"], ["g_sq"])
                    pb, pk = pbank()
                    for c2 in range(2):
                        for c3 in range(3):
                            tr(pb[:, (c3 * 2 + c2) * 64:(c3 * 2 + c2 + 1) * 64],
                               fl("sq")[:, c2 * 384 + c3 * 128:c2 * 384 + (c3 + 1) * 128], ident[0:64, 0:64], ["g_sq", "cst"], [pk])
                    act(yT[:, 5:8, cp2 * 128:(cp2 + 1) * 128], pb[:, 0:384].rearrange("p (c t) -> p c t", c=3), AF.Copy, [pk], ["yT"])

                for mi, (a0, a1) in enumerate(((0, 2), (2, 5), (5, 8))):
                    if not ymask[mi]:
                        memset("dve", yT[:, a0:a1, :], 0.0, ["yT"])
                for i in range(2):
                    t = t0 + i
                    for hf in range(2):
                        pb, pk = pbank()
                        for kc in range(8):
                            mm(pb[:, :], yT[:, kc, i * 128:(i + 1) * 128], wout[:, kc, hf * 512:(hf + 1) * 512], kc == 0,
                               kc == 7, ["yT"] + woutk, [pk])
                        stt("dve", zz[:, hf * 512:(hf + 1) * 512], xg[:, i, hf * 512:(hf + 1) * 512], ALPHA, pb[:, :],
                            ALU.mult, ALU.add, [("xg", i), pk], ["zz"])
                    layer_norm_tile(S, zz, xg[:, i, :], lnb[:, 0, :], lnb[:, 1, :], stats, mv, ("xg", i))
                    S.dma("sp", xs[t * 128:(t + 1) * 128, :], xg[:, i, :], reads=[("xg", i)], writes=[("xs", t)])
            S.barrier()

            if not do_moe:
                continue
            base[0] = phase_base
            xres = alloc("xres", [128, NT, D], F32)
            for t in range(NT):
                S.dma("sp", xres[:, t, :], xs[t * 128:(t + 1) * 128, :], reads=[("xs", t)], writes=[("x", t)])
            h2T = alloc("h2T", [128, 8, T], BF16)
            Gt = alloc("Gt", [128, NT, NE], F32)
            stats2 = alloc("stats2", [128, 2, 6], F32)
            mv2 = alloc("mv2", [128, 2], F32)
            moe_mark = base[0]
            h32 = alloc("h32", [128, 8, 128], F32)
            rw = alloc("rw", [128, 8, NE], F32)
            lg = alloc("lg", [128, NE], F32)
            mx8 = alloc("mx8", [128, 8], F32)
            rbb = alloc("rbb", [128, NE], F32)
            gsum = alloc("gsum", [128, 1], F32)
            bdn = alloc("bdn", [NE, D], F32)
            bdnb = alloc("bdnb", [NE, D], BF16)
            GTb = alloc("GTb", [NE, T], BF16)

            S.dma("sp", rw[:], dr["router_w"][l, :, :].rearrange("(k p) n -> p k n", p=128), writes=["rw"])
            bcast_row(rbb[:, :], dr["router_b"][l:l + 1, :], NE, "rbb")
            S.dma("sp", bdn[:], dr["exp_b_down"][l, :, :], writes=["bdn"])
            tt("dve", bdnb[:], bdn[:], gbc[0:NE, 1, :], ALU.mult, ["bdn", "gbc"], ["bdnb"])
            for t in range(NT):
                for kc in range(8):
                    if kc % 4 == 0:
                        pb, pk = pbank()
                    tr(pb[:, (kc % 4) * 128:(kc % 4 + 1) * 128], xres[:, t, kc * 128:(kc + 1) * 128], ident,
                       [("x", t), "cst"], [pk])
                    act(h2T[:, kc, t * 128:(t + 1) * 128], pb[:, (kc % 4) * 128:(kc % 4 + 1) * 128], AF.Identity,
                        [pk, "modcol"], ["h2T"], scale=modcol[:, 3, kc:kc + 1], bias=modcol[:, 2, kc:kc + 1])
                    act(h32[:, kc, :], pb[:, (kc % 4) * 128:(kc % 4 + 1) * 128], AF.Identity,
                        [pk, "modcol"], ["h32"], scale=modcol[:, 3, kc:kc + 1], bias=modcol[:, 2, kc:kc + 1])
                pb, pk = pbank()
                for kc in range(8):
                    mm(pb[:, 0:NE], h32[:, kc, :], rw[:, kc, :], kc == 0, kc == 7, ["h32", "rw"], [pk])
                tt("dve", lg[:], pb[:, 0:NE], rbb[:], ALU.add, [pk, "rbb"], ["lg"])
                S.op("dve", lambda e: e.max(out=mx8[:], in_=lg[:]), ["lg"], ["mx8"])
                ts("dve", Gt[:, t, :], lg[:], mx8[:, 3:4], None, ALU.is_ge, None, ["lg", "mx8"], [("Gt", t)])
                ts("dve", lg[:], lg[:], mx8[:, 0:1], None, ALU.subtract, None, ["lg", "mx8"], ["lg"])
                act(lg[:], lg[:], AF.Exp, ["lg"], ["lg"])
                tt("dve", Gt[:, t, :], Gt[:, t, :], lg[:], ALU.mult, [("Gt", t), "lg"], [("Gt", t)])
                S.op("dve", lambda e, t=t: e.tensor_reduce(out=gsum[:], in_=Gt[:, t, :], axis=AX.X, op=ALU.add),
                     [("Gt", t)], ["gsum"])
                S.op("dve", lambda e: e.reciprocal(out=gsum[:], in_=gsum[:]), ["gsum"], ["gsum"])
                ts("dve", Gt[:, t, :], Gt[:, t, :], gsum[:, 0:1], None, ALU.mult, None, [("Gt", t), "gsum"], [("Gt", t)])
                pb, pk = pbank()
                tr(pb[0:NE, 0:128], Gt[:, t, :], ident, [("Gt", t), "cst"], [pk])
                act(GTb[:, t * 128:(t + 1) * 128], pb[0:NE, 0:128], AF.Copy, [pk], ["GTb"])
                for hf in range(2):
                    pb, pk = pbank()
                    mm(pb[:, :], GTb[:, t * 128:(t + 1) * 128], bdnb[:, hf * 512:(hf + 1) * 512], True, True,
                       ["GTb", "bdnb"], [pk])
                    stt("dve", xres[:, t, hf * 512:(hf + 1) * 512], xres[:, t, hf * 512:(hf + 1) * 512], ALPHA, pb[:, :],
                        ALU.mult, ALU.add, [("x", t), pk, "h2T", "h32"], [("x", t)])
            S.barrier()
            base[0] = moe_mark
            NUPR = 2
            wupr = [alloc("wupr%d" % i, [128, 8, 256], BF16) for i in range(NUPR)]
            wdnr = [alloc("wdnr%d" % i, [128, 8, 512], BF16) for i in range(2)]
            bup = alloc("bup", [128, 2, 16], F32)
            glb = [alloc("gl%d" % i, [128, 512], F32) for i in range(2)]
            sgb = [alloc("sg%d" % i, [128, 512], F32) for i in range(2)]
            llb = [alloc("ll%d" % i, [128, 512], F32) for i in range(2)]
            actT = alloc("actT", [128, 8, T], BF16)
            stg["bufs"] = [alloc("stgA", [128, 8, 256], F32), alloc("stgB", [128, 8, 256], F32)]
            ts("dve", gbc[:, 1, :], gbc[:, 1, :], 1.0 / 1.702, None, ALU.mult, None, ["gbc", "bdnb"], ["gbc"])
            NTG = T // 512
            ui = 0
            di = 0
            gi_ = 0
            for e in range(n_exp):
                bslot = e % 2
                S.dma("sp", bup[:, bslot, :], dr["exp_b_up"][l, e, :].rearrange("(c p) -> p c", p=128),
                      writes=[("bup", bslot)], slow=True)
                ts("pool", bup[:, bslot, 8:16], bup[:, bslot, 8:16], 1.0, None, ALU.add, None, [("bup", bslot)], [("bup", bslot)])
                pending = None
                for fc in range(8):
                    wu = wupr[ui % NUPR]
                    wk = "wupr%d" % (ui % NUPR)
                    ui += 1
                    wks = load_cast(wu[:, :, :], None, 256, wk, srcs=[
                        (0, 128, dr["exp_w_up"][l, e, :, fc * 128:(fc + 1) * 128]),
                        (128, 128, dr["exp_w_up"][l, e, :, D + fc * 128:D + (fc + 1) * 128])])
                    for tg in range(NTG):
                        b2 = gi_ % 2
                        gi_ += 1
                        gl, sg, ll = glb[b2], sgb[b2], llb[b2]
                        pbg, pkg = pbank()
                        pbl, pkl = pbank()
                        for kc in range(8):
                            mm(pbg[:, :], wu[:, kc, 0:128], h2T[:, kc, tg * 512:(tg + 1) * 512], kc == 0, kc == 7,
                               wks + ["h2T"], [pkg])
                        for kc in range(8):
                            mm(pbl[:, :], wu[:, kc, 128:256], h2T[:, kc, tg * 512:(tg + 1) * 512], kc == 0, kc == 7,
                               wks + ["h2T"], [pkl])
                        ts("dve", gl[:], pbg[:, :], bup[:, bslot, fc:fc + 1], 7.0, ALU.add, ALU.min,
                           [pkg, ("bup", bslot)], ["gl%d" % b2])
                        ts("dve", ll[:], pbl[:, :], bup[:, bslot, 8 + fc:9 + fc], 8.0, ALU.add, ALU.min,
                           [pkl, ("bup", bslot)], ["ll%d" % b2])
                        act(sg[:], gl[:], AF.Silu, ["gl%d" % b2], ["sg%d" % b2], scale=1.702)
                        if pending is not None:
                            pfc, ptg, pb2 = pending
                            stt("dve", actT[:, pfc, ptg * 512:(ptg + 1) * 512], llb[pb2][:], -6.0, sgb[pb2][:], ALU.max, ALU.mult,
                                ["ll%d" % pb2, "sg%d" % pb2], [("actT", ptg)])
                        pending = (fc, tg, b2)
                pfc, ptg, pb2 = pending
                stt("dve", actT[:, pfc, ptg * 512:(ptg + 1) * 512], llb[pb2][:], -6.0, sgb[pb2][:], ALU.max, ALU.mult,
                    ["ll%d" % pb2, "sg%d" % pb2], [("actT", ptg)])
                for hf in range(2):
                    wd = wdnr[di % 2]
                    dk = "wdnr%d" % (di % 2)
                    di += 1
                    dks = []
                    for c0 in range(0, 512, 256):
                        dks += load_cast(wd[:, :, c0:c0 + 256], dr["exp_w_down"][l, e, :, hf * 512 + c0:hf * 512 + c0 + 256], 256,
                                         (dk, c0), mul=gbc[:, 1, hf * 512 + c0:hf * 512 + c0 + 256])
                    for t in range(NT):
                        pb, pk = pbank()
                        for fc in range(8):
                            mm(pb[:, :], actT[:, fc, t * 128:(t + 1) * 128], wd[:, fc, :], fc == 0, fc == 7,
                               [("actT", t // 4)] + dks, [pk])
                        stt("dve", xres[:, t, hf * 512:(hf + 1) * 512], pb[:, :], Gt[:, t, e:e + 1],
                            xres[:, t, hf * 512:(hf + 1) * 512], ALU.mult, ALU.add, [pk, ("Gt", t), ("x", t)], [("x", t)])
            S.barrier()
            base[0] = moe_mark
            lnb = alloc("lnb2", [128, 2, D], F32)
            for i, nm in enumerate(("ln2_g", "ln2_b")):
                bcast_row(lnb[:, i, :], dr[nm][l:l + 1, :], 1024, "lnb")
            dst = out if l == NL - 1 else xs
            for t in range(NT):
                layer_norm_tile(S, xres[:, t, :], xres[:, t, :], lnb[:, 0, :], lnb[:, 1, :], stats2, mv2, ("x", t), key=("x", t))
                S.dma("sp", dst[t * 128:(t + 1) * 128, :], xres[:, t, :], reads=[("x", t)], writes=[("xs", t)])
            S.barrier()
        if not do_moe:
            base[0] = phase_base
            xg2 = alloc("xg2", [128, D], F32)
            for t in range(NT):
                S.dma("sp", xg2[:], xs[t * 128:(t + 1) * 128, :], reads=[("xs", t)], writes=["xg2"])
                S.dma("sp", out[t * 128:(t + 1) * 128, :], xg2[:], reads=["xg2"])
        S.emit()
    return nc


def layer_norm_tile(S, zz, dst, gam, bet, stats, mv, dkey, key="zz"):
    sk = "st_" + str(key)
    mk = "mv_" + str(key)
    for hf in range(2):
        S.op("dve", lambda e, hf=hf: e.bn_stats(out=stats[:, hf, :], in_=zz[:, hf * 512:(hf + 1) * 512]), [key], [sk + str(hf)])
    S.op("dve", lambda e: e.bn_aggr(out=mv[:], in_=stats[:].rearrange("p a b -> p (a b)")), [sk + "0", sk + "1"], [mk])
    S.op("act", lambda e: e.activation(out=mv[:, 1:2], in_=mv[:, 1:2], func=AF.Sqrt, bias=1e-5), [mk], [mk])
    S.op("dve", lambda e: e.reciprocal(out=mv[:, 1:2], in_=mv[:, 1:2]), [mk], [mk])
    S.op("dve", lambda e: e.tensor_scalar(out=zz[:], in0=zz[:], scalar1=mv[:, 0:1], scalar2=mv[:, 1:2], op0=ALU.subtract,
                                          op1=ALU.mult), [key, mk], [key])
    S.op("pool", lambda e: e.tensor_tensor(out=zz[:], in0=zz[:], in1=gam, op=ALU.mult), [key, "lnb"], [key])
    S.op("pool", lambda e: e.tensor_tensor(out=dst, in0=zz[:], in1=bet, op=ALU.add), [key, "lnb"], [dkey])


_NC_CACHE = {}


def kernel(**inputs):
    x = np.ascontiguousarray(inputs["x"], dtype=np.float32)
    B, T, _ = x.shape
    NG = T // 256
    key = (NG,)
    if key not in _NC_CACHE:
        _NC_CACHE[key] = build(NG=NG)
    nc = _NC_CACHE[key]
    consts = make_consts()
    c = np.ascontiguousarray(inputs["c"], dtype=np.float32)
    shared = {nm: np.ascontiguousarray(inputs[nm], dtype=np.float32) for nm, _ in WNAMES}
    in_maps = []
    for b in range(B):
        m = {"x": x[b], "c": c[b:b + 1], "consts": consts}
        m.update(shared)
        in_maps.append(m)
    res = run_bass_kernel_spmd(nc, in_maps, core_ids=list(range(B)))
    return np.stack([r["out"] for r in res.results], axis=0).astype(np.float32)
```
